# Optimizing a Trainium2 kernel written in Bass

```python
import math
import jax
import jax.numpy as jnp
from jax import lax
import numpy as np

D_MODEL = 1024
BATCH = 8
SEQ = 2048
DEPTH = 2

CTX_LEN = 256
GRID_W = 64
D_MIX = D_MODEL
EPS = 1e-6
F32 = jnp.float32

LRU_WIDTH = D_MIX // 4
LRU_HEADS = 4
LRU_HD = LRU_WIDTH // LRU_HEADS
CONV_W = 4
RG_C = 8.0
DA_WIDTH = D_MIX // 4
DA_HEADS = 4
DA_VD = DA_WIDTH // DA_HEADS
DA_QD = DA_VD // 2
ROPE_THETA = 10000.0
Q_BLOCK = 128
SG_WIDTH = D_MIX // 4
SG_HEADS = 4
SG_HD = SG_WIDTH // SG_HEADS
SG_CHUNK = 128
HG_WIDTH = D_MIX // 4
HG_HEADS = 4
HG_HD = HG_WIDTH // HG_HEADS
HG_CHUNK = 64
N_EXPERTS = 16
N_GROUPS = 4
EXP_PER_GROUP = N_EXPERTS // N_GROUPS
TOP_K = 2
D_EXPERT = 512

IN_COLS = 2 * LRU_WIDTH + 3 * DA_WIDTH + 2 * SG_WIDTH + 5 * HG_WIDTH
SPLITS = (2 * LRU_WIDTH, 2 * LRU_WIDTH + 3 * DA_WIDTH, 2 * LRU_WIDTH + 3 * DA_WIDTH + 2 * SG_WIDTH)

kernel_name = 'hybrid_parallel_heads_diffusion_block'


def rms_norm(x, g):
    xf = x.astype(F32)
    y = xf * lax.rsqrt(jnp.mean(xf * xf, axis=-1, keepdims=True) + EPS)
    return (y * g.astype(F32)).astype(x.dtype)


def modulate(h, shift, scale):
    return h * (1.0 + scale) + shift


def tflip(t, rev):
    return jnp.flip(t, axis=1) if rev else t


def centred_dwconv(x, w, b):
    T = x.shape[1]
    left = CONV_W // 2
    xp = jnp.pad(x, ((0, 0), (left, CONV_W - 1 - left), (0, 0)))
    return sum(xp[:, j:j + T] * w[j] for j in range(CONV_W)) + b


def block_diag(x, w, b):
    B_, T, _ = x.shape
    xh = x.reshape(B_, T, LRU_HEADS, LRU_HD)
    return jnp.einsum('bthi,hij->bthj', xh, w).reshape(B_, T, LRU_WIDTH) + b


def rglru_coeffs(u, w_r, b_r, w_i, b_i, lam):
    r = jax.nn.sigmoid(block_diag(u, w_r, b_r).astype(F32))
    i = jax.nn.sigmoid(block_diag(u, w_i, b_i).astype(F32))
    log_a = -RG_C * r * jax.nn.softplus(-lam.astype(F32))
    b = jnp.sqrt(-jnp.expm1(2.0 * log_a)) * i * u.astype(F32)
    return log_a, b


def _lin_combine(e1, e2):
    a1, b1 = e1
    a2, b2 = e2
    return a1 * a2, a2 * b1 + b2


def lru_scan(log_a, b, h0):
    A, Bc = lax.associative_scan(_lin_combine, (jnp.exp(log_a), b), axis=1)
    return A * h0[:, None] + Bc


def lru_final(log_a, b):
    c = jnp.cumsum(log_a, axis=1)
    return jnp.sum(jnp.exp(c[:, -1:] - c) * b, axis=1)


def rglru_mixer(z, zc, conv_w, conv_b, w_r, b_r, w_i, b_i, lam, ctx_out):
    xa, ga = jnp.split(z, 2, axis=-1)
    xca, gca = jnp.split(zc, 2, axis=-1)
    u = centred_dwconv(xa, conv_w, conv_b)
    uc = centred_dwconv(xca, conv_w, conv_b)
    h_sum, hc_sum = 0.0, 0.0
    for d in range(2):
        rev = d == 1
        la_c, b_c = rglru_coeffs(tflip(uc, rev), w_r[d], b_r[d], w_i[d], b_i[d], lam[d])
        if ctx_out:
            hc = lru_scan(la_c, b_c, jnp.zeros_like(b_c[:, 0]))
            hc_last = hc[:, -1]
            hc_sum = hc_sum + tflip(hc, rev)
        else:
            hc_last = lru_final(la_c, b_c)
        la, b = rglru_coeffs(tflip(u, rev), w_r[d], b_r[d], w_i[d], b_i[d], lam[d])
        h_sum = h_sum + tflip(lru_scan(la, b, hc_last), rev)
    y = (h_sum * jax.nn.gelu(ga.astype(F32))).astype(z.dtype)
    yc = (hc_sum * jax.nn.gelu(gca.astype(F32))).astype(z.dtype) if ctx_out else None
    return y, yc


def rope_2d(t, cos, sin):
    sh = t.shape
    tr = t.reshape(sh[:-1] + (2, 2, DA_QD // 4))
    x1, x2 = tr[..., 0, :], tr[..., 1, :]
    c = cos[None, :, None, None]
    s = sin[None, :, None, None]
    out = jnp.stack([x1 * c - x2 * s, x2 * c + x1 * s], axis=-2)
    return out.reshape(sh)


def diff_attend(q, k, v, lam):
    s = jnp.einsum('bqhnd,bkhnd->bnhqk', q, k).astype(F32) * (DA_QD ** -0.5)
    p = jax.nn.softmax(s, axis=-1)
    pd = p[:, 0] - lam * p[:, 1]
    return jnp.einsum('bhqk,bkhe->bqhe', pd.astype(v.dtype), v)


def diff_attn_mixer(z, zc, lam_vecs, sub_g, lam_init, cos, sin, ctx_out):
    B_, T, _ = z.shape
    Tc = zc.shape[1]
    q, k, v = jnp.split(z, 3, axis=-1)
    qc, kc, vc = jnp.split(zc, 3, axis=-1)
    q = rope_2d(q.reshape(B_, T, DA_HEADS, 2, DA_QD), cos, sin)
    k = rope_2d(k.reshape(B_, T, DA_HEADS, 2, DA_QD), cos, sin)
    v = v.reshape(B_, T, DA_HEADS, DA_VD)
    kc = kc.reshape(B_, Tc, DA_HEADS, 2, DA_QD)
    vc = vc.reshape(B_, Tc, DA_HEADS, DA_VD)
    lf = lam_vecs.astype(F32)
    lam = jnp.exp(jnp.sum(lf[0] * lf[1])) - jnp.exp(jnp.sum(lf[2] * lf[3])) + lam_init
    K = jnp.concatenate([kc, k], axis=1)
    V = jnp.concatenate([vc, v], axis=1)
    nb = T // Q_BLOCK
    qb = jnp.moveaxis(q.reshape(B_, nb, Q_BLOCK, DA_HEADS, 2, DA_QD), 1, 0)
    o = lax.map(lambda blk: diff_attend(blk, K, V, lam), qb)
    o = jnp.moveaxis(o, 0, 1).reshape(B_, T, DA_HEADS, DA_VD)
    y = (rms_norm(o, sub_g) * (1.0 - lam_init)).reshape(B_, T, DA_WIDTH).astype(z.dtype)
    yc = None
    if ctx_out:
        oc = diff_attend(qc.reshape(B_, Tc, DA_HEADS, 2, DA_QD), kc, vc, lam)
        yc = (rms_norm(oc, sub_g) * (1.0 - lam_init)).reshape(B_, Tc, DA_WIDTH).astype(z.dtype)
    return y, yc


def spatial_gating(z, norm_g, w_sp, b_sp):
    B_, T, _ = z.shape
    u, v = jnp.split(jax.nn.gelu(z), 2, axis=-1)
    v = rms_norm(v, norm_g).reshape(B_, T // SG_CHUNK, SG_CHUNK, SG_HEADS, SG_HD)
    vm = jnp.einsum('hpq,bnqhc->bnphc', w_sp, v) + b_sp.T[None, None, :, :, None]
    return u * vm.reshape(B_, T, SG_WIDTH)


def hgrn_gates(f_pre, lb):
    f = lb + (1.0 - lb) * jax.nn.sigmoid(f_pre.astype(F32))
    return jnp.log(f), 1.0 - f


def gla_scan(q, k, v, log_f, S0):
    B_, T, H, Dk = q.shape
    nc = T // HG_CHUNK
    to_chunks = lambda t: jnp.transpose(t.reshape(B_, nc, HG_CHUNK, H, t.shape[-1]), (1, 0, 3, 2, 4))
    mask = jnp.tril(jnp.ones((HG_CHUNK, HG_CHUNK), dtype=bool))[:, :, None]

    def step(S, inp):
        qc, kc, vc, lfc = inp
        b = jnp.cumsum(lfc, axis=2)
        diff = b[:, :, :, None, :] - b[:, :, None, :, :]
        dec = jnp.exp(jnp.where(mask, diff, -jnp.inf))
        att = jnp.einsum('bhtd,bhsd,bhtsd->bhts', qc, kc, dec)
        o = att @ vc + jnp.einsum('bhtd,bhdv->bhtv', qc * jnp.exp(b), S)
        b_last = b[:, :, -1:]
        S_new = jnp.exp(b_last[:, :, 0])[..., None] * S + jnp.einsum('bhsd,bhsv->bhdv', kc * jnp.exp(b_last - b), vc)
        return S_new, o

    S_T, o = lax.scan(step, S0, (to_chunks(q), to_chunks(k), to_chunks(v), to_chunks(log_f)))
    o = jnp.transpose(o, (1, 0, 3, 2, 4)).reshape(B_, T, H, v.shape[-1])
    return o, S_T


def gla_final(k, v, log_f):
    c = jnp.cumsum(log_f, axis=1)
    return jnp.einsum('bthd,bthv->bhdv', k * jnp.exp(c[:, -1:] - c), v)


def hgrn2_mixer(z, zc, lb, norm_g, ctx_out):
    heads = lambda t: t.reshape(t.shape[0], t.shape[1], HG_HEADS, HG_HD).astype(F32)
    q, f_fw, f_bw, i, g = jnp.split(z, 5, axis=-1)
    qc, fc_fw, fc_bw, ic, gc = jnp.split(zc, 5, axis=-1)
    q = heads(q) * (HG_HD ** -0.5)
    i = heads(i)
    ic = heads(ic)
    B_ = z.shape[0]
    o_sum, oc_sum = 0.0, 0.0
    for d, (fl, fcl) in enumerate(((f_fw, fc_fw), (f_bw, fc_bw))):
        rev = d == 1
        lb_h = lb[d].reshape(HG_HEADS, HG_HD)
        lf_c, k_c = hgrn_gates(tflip(heads(fcl), rev), lb_h)
        v_c = tflip(ic, rev)
        if ctx_out:
            S0 = jnp.zeros((B_, HG_HEADS, HG_HD, HG_HD), F32)
            o_c, S_c = gla_scan(tflip(heads(qc) * (HG_HD ** -0.5), rev), k_c, v_c, lf_c, S0)
            oc_sum = oc_sum + tflip(o_c, rev)
        else:
            S_c = gla_final(k_c, v_c, lf_c)
        lf, k = hgrn_gates(tflip(heads(fl), rev), lb_h)
        o, _ = gla_scan(tflip(q, rev), k, tflip(i, rev), lf, S_c)
        o_sum = o_sum + tflip(o, rev)
    gn = norm_g.reshape(HG_HEADS, HG_HD)
    y = (rms_norm(o_sum, gn).reshape(z.shape[0], z.shape[1], HG_WIDTH) * jax.nn.silu(g.astype(F32))).astype(z.dtype)
    yc = None
    if ctx_out:
        yc = (rms_norm(oc_sum, gn).reshape(zc.shape[0], zc.shape[1], HG_WIDTH) * jax.nn.silu(gc.astype(F32))).astype(z.dtype)
    return y, yc


def moe(h, w_router, b_router, w1, w3, w2):
    logits = (h @ w_router).astype(F32) + b_router.astype(F32)
    scores = jax.nn.softmax(logits, axis=-1)
    grp = scores.reshape(scores.shape[:-1] + (N_GROUPS, EXP_PER_GROUP))
    grp_score = jnp.sum(lax.top_k(grp, TOP_K)[0], axis=-1)
    g_sel = jnp.argmax(grp_score, axis=-1)
    in_group = g_sel[..., None] == (jnp.arange(N_EXPERTS) // EXP_PER_GROUP)
    masked = jnp.where(in_group, scores, -jnp.inf)
    top_w, top_i = lax.top_k(masked, TOP_K)
    top_w = top_w / jnp.sum(top_w, axis=-1, keepdims=True)
    gate = jnp.sum(jax.nn.one_hot(top_i, N_EXPERTS, dtype=F32) * top_w[..., None], axis=-2)
    out = 0.0
    for e in range(N_EXPERTS):
        he = jax.nn.silu(h @ w1[e]) * (h @ w3[e])
        out = out + gate[..., e:e + 1].astype(h.dtype) * (he @ w2[e])
    return out


def setup_inputs(seed: int = 0) -> dict:
    key = jax.random.key(seed)
    ks = iter(jax.random.split(key, 40))
    nrm = lambda shape, s: s * jax.random.normal(next(ks), shape, F32)
    L = DEPTH
    a_pow = jax.random.uniform(next(ks), (L, 2, LRU_WIDTH), F32, minval=0.9, maxval=0.999)
    a0 = a_pow ** (1.0 / RG_C)
    lru_lam = jnp.log(a0) - jnp.log1p(-a0)
    return {
        'x': nrm((BATCH, SEQ, D_MODEL), 1.0),
        'c': nrm((BATCH, D_MODEL), 1.0),
        'ctx': nrm((BATCH, CTX_LEN, D_MODEL), 1.0),
        'c_ctx': nrm((D_MODEL,), 1.0),
        'w_ada': nrm((L, D_MODEL, 6 * D_MODEL), 0.5 * D_MODEL ** -0.5),
        'b_ada': nrm((L, 6 * D_MODEL), 0.02),
        'norm1_g': 1.0 + nrm((L, D_MODEL), 0.02),
        'norm2_g': 1.0 + nrm((L, D_MODEL), 0.02),
        'w_in': nrm((L, D_MODEL, IN_COLS), D_MODEL ** -0.5),
        'w_out': nrm((L, D_MIX, D_MODEL), D_MIX ** -0.5),
        'lru_conv_w': nrm((L, CONV_W, LRU_WIDTH), CONV_W ** -0.5),
        'lru_conv_b': nrm((L, LRU_WIDTH), 0.02),
        'lru_wr': nrm((L, 2, LRU_HEADS, LRU_HD, LRU_HD), LRU_HD ** -0.5),
        'lru_br': nrm((L, 2, LRU_WIDTH), 0.02),
        'lru_wi': nrm((L, 2, LRU_HEADS, LRU_HD, LRU_HD), LRU_HD ** -0.5),
        'lru_bi': nrm((L, 2, LRU_WIDTH), 0.02),
        'lru_lam': lru_lam,
        'da_lam': nrm((L, 4, DA_QD), 0.1),
        'da_subln_g': 1.0 + nrm((L, DA_VD), 0.02),
        'sg_norm_g': 1.0 + nrm((L, SG_WIDTH), 0.02),
        'sg_w': nrm((L, SG_HEADS, SG_CHUNK, SG_CHUNK), SG_CHUNK ** -0.5),
        'sg_b': 1.0 + nrm((L, SG_HEADS, SG_CHUNK), 0.02),
        'hg_lb': nrm((2, L, HG_WIDTH), 0.1),
        'hg_norm_g': 1.0 + nrm((L, HG_WIDTH), 0.02),
        'router_w': nrm((D_MODEL, N_EXPERTS), D_MODEL ** -0.5),
        'router_b': nrm((N_EXPERTS,), 0.01),
        'moe_w1': nrm((L, N_EXPERTS, D_MODEL, D_EXPERT), D_MODEL ** -0.5),
        'moe_w3': nrm((L, N_EXPERTS, D_MODEL, D_EXPERT), D_MODEL ** -0.5),
        'moe_w2': nrm((L, N_EXPERTS, D_EXPERT, D_MODEL), D_EXPERT ** -0.5),
        'final_norm_g': 1.0 + nrm((D_MODEL,), 0.02),
    }


def reference(x, c, ctx, c_ctx, w_ada, b_ada, norm1_g, norm2_g, w_in, w_out,
              lru_conv_w, lru_conv_b, lru_wr, lru_br, lru_wi, lru_bi, lru_lam,
              da_lam, da_subln_g, sg_norm_g, sg_w, sg_b, hg_lb, hg_norm_g,
              router_w, router_b, moe_w1, moe_w3, moe_w2, final_norm_g):
    seq = x.shape[1]
    rows = seq // GRID_W
    row_ids = jnp.repeat(jnp.arange(rows, dtype=F32), GRID_W)
    col_ids = jnp.tile(jnp.arange(GRID_W, dtype=F32), rows)
    n_freq = DA_QD // 4
    freqs = ROPE_THETA ** (-jnp.arange(n_freq, dtype=F32) / n_freq)
    ang = jnp.stack([row_ids[:, None] * freqs, col_ids[:, None] * freqs], axis=1)
    cos = jnp.cos(ang).astype(x.dtype)
    sin = jnp.sin(ang).astype(x.dtype)
    lb_cum = jnp.cumsum(jax.nn.softmax(hg_lb.astype(F32), axis=1), axis=1)
    lb_all = lb_cum - lb_cum[:, :1]
    silu_c = jax.nn.silu(c)
    silu_cc = jax.nn.silu(c_ctx)
    xc = ctx
    for l in range(DEPTH):
        ctx_out = l < DEPTH - 1
        mod = (silu_c @ w_ada[l] + b_ada[l])[:, None, :]
        mod_c = silu_cc @ w_ada[l] + b_ada[l]
        sh1, sc1, gt1, sh2, sc2, gt2 = jnp.split(mod, 6, axis=-1)
        shc1, scc1, gtc1, shc2, scc2, gtc2 = jnp.split(mod_c, 6, axis=-1)
        h = modulate(rms_norm(x, norm1_g[l]), sh1, sc1)
        hc = modulate(rms_norm(xc, norm1_g[l]), shc1, scc1)
        za, zb, zs, zh = jnp.split(h @ w_in[l], SPLITS, axis=-1)
        zca, zcb, zcs, zch = jnp.split(hc @ w_in[l], SPLITS, axis=-1)
        ya, yca = rglru_mixer(za, zca, lru_conv_w[l], lru_conv_b[l], lru_wr[l], lru_br[l],
                              lru_wi[l], lru_bi[l], lru_lam[l], ctx_out)
        lam_init = 0.8 - 0.6 * math.exp(-0.3 * l)
        yb, ycb = diff_attn_mixer(zb, zcb, da_lam[l], da_subln_g[l], lam_init, cos, sin, ctx_out)
        ys = spatial_gating(zs, sg_norm_g[l], sg_w[l], sg_b[l])
        yh, ych = hgrn2_mixer(zh, zch, lb_all[:, l], hg_norm_g[l], ctx_out)
        x = x + gt1 * (jnp.concatenate([ya, yb, ys, yh], axis=-1) @ w_out[l])
        h2 = modulate(rms_norm(x, norm2_g[l]), sh2, sc2)
        x = x + gt2 * moe(h2, router_w, router_b, moe_w1[l], moe_w3[l], moe_w2[l])
        if ctx_out:
            ycs = spatial_gating(zcs, sg_norm_g[l], sg_w[l], sg_b[l])
            xc = xc + gtc1 * (jnp.concatenate([yca, ycb, ycs, ych], axis=-1) @ w_out[l])
            hc2 = modulate(rms_norm(xc, norm2_g[l]), shc2, scc2)
            xc = xc + gtc2 * moe(hc2, router_w, router_b, moe_w1[l], moe_w3[l], moe_w2[l])
    return rms_norm(x, final_norm_g)
```

```python
import contextlib
import math
import numpy as np
import concourse.bass as bass
import concourse.mybir as mybir
from concourse.bass_utils import run_bass_kernel_spmd

F32 = mybir.dt.float32
BF16 = mybir.dt.bfloat16
AF = mybir.ActivationFunctionType
ALU = mybir.AluOpType
AX = mybir.AxisListType

D = 1024
T = 2048
TC = 256
TT = T + TC
NT = TT // 128
DEPTH = 2
EPS = 1e-6
NE = 16
DE = 512
import os as _os
KCUT = int(_os.environ.get('KCUT', '0'))


class _Eng:
    def __init__(self, S, name, h):
        self.S = S
        self.name = name
        self.h = h
        self.sem = S.es.enter_context(S.nc.semaphore("sem_" + name))
        self.count = 0
        self.seen = {}


class _DSem:
    def __init__(self, S, name):
        self.name = name
        self.sem = S.es.enter_context(S.nc.semaphore(name))
        self.count = 0


class Sched:
    def __init__(self, nc, es):
        self.nc = nc
        self.es = es
        self.E = {
            "pe": _Eng(self, "pe", nc.tensor),
            "act": _Eng(self, "act", nc.scalar),
            "dve": _Eng(self, "dve", nc.vector),
            "pool": _Eng(self, "pool", nc.gpsimd),
            "sp": _Eng(self, "sp", nc.sync),
        }
        self.last_w = {}
        self.reads = {}
        self.dsems = {q: [_DSem(self, "dsem_%s_%d" % (q, i)) for i in range(12)] for q in ("sp", "act", "pool")}
        self.dnext = {q: 0 for q in self.dsems}
        self.n_inst = 0
        self.nrot = 0
        self.rec = None

    def _need(self, eng, deps):
        for obj, c in deps.items():
            if c <= 0:
                continue
            if obj is eng and eng.name in ("pe", "sp"):
                continue
            if eng.seen.get(obj, 0) >= c:
                continue
            eng.h.wait_ge(obj.sem, c)
            eng.seen[obj] = c

    def _deps(self, r, w):
        deps = {}
        for k in r:
            lw = self.last_w.get(k)
            if lw is not None:
                deps[lw[0]] = max(deps.get(lw[0], 0), lw[1])
        for k in w:
            lw = self.last_w.get(k)
            if lw is not None:
                deps[lw[0]] = max(deps.get(lw[0], 0), lw[1])
            for o, c in self.reads.get(k, {}).items():
                deps[o] = max(deps.get(o, 0), c)
        return deps

    def _mark(self, obj, cnt, r, w):
        for k in r:
            self.reads.setdefault(k, {})[obj] = cnt
        for k in w:
            self.last_w[k] = (obj, cnt)
            self.reads[k] = {}

    def op(self, en, fn, r=(), w=()):
        if self.rec is not None:
            self.rec.append((en, fn, tuple(r), tuple(w)))
            return None
        eng = self.E[en]
        self._need(eng, self._deps(r, w))
        inst = fn(eng.h)
        eng.count += 1
        inst.then_inc(eng.sem, 1)
        self._mark(eng, eng.count, r, w)
        self.n_inst += 1
        return inst

    def dma(self, q, out, in_, r=(), w=()):
        eng = self.E[q]
        deps = self._deps(r, w)
        lst = self.dsems[q]
        ds = lst[self.dnext[q] % len(lst)]
        self.dnext[q] += 1
        if ds.count:
            deps[ds] = max(deps.get(ds, 0), ds.count)
        self._need(eng, deps)
        inst = eng.h.dma_start(out=out, in_=in_)
        ds.count += 16
        inst.then_inc(ds.sem, 16)
        self._mark(ds, ds.count, r, w)
        self.n_inst += 1
        return inst

    def record(self, f):
        assert self.rec is None
        self.rec = []
        f()
        lst, self.rec = self.rec, None
        return lst

    def seg(self):
        if self.rec is not None:
            self.rec.append(None)

    @staticmethod
    def split(l):
        cur, out = [], []
        for it in l:
            if it is None:
                if cur:
                    out.append(cur)
                cur = []
            else:
                cur.append(it)
        if cur:
            out.append(cur)
        return out

    def emit_interleaved(self, lists):
        self.emit_segs([self.split(l) for l in lists])

    def emit_segs(self, segs):
        k = 0
        while any(k < len(s) for s in segs):
            for s in segs:
                if k < len(s):
                    for en, fn, r, w in s[k]:
                        self.op(en, fn, r, w)
            k += 1

    def barrier(self):
        objs = list(self.E.values())
        dl = [d for lst in self.dsems.values() for d in lst if d.count]
        for e in objs:
            deps = {o: o.count for o in objs if o is not e}
            for d in dl:
                deps[d] = d.count
            self._need(e, deps)
        self.last_w = {}
        self.reads = {}
        for e in objs:
            if e.count > 3000:
                self.nrot += 1
                e.sem = self.es.enter_context(self.nc.semaphore("sem_%s_r%d" % (e.name, self.nrot)))
                e.count = 0
                for o in objs:
                    o.seen.pop(e, None)

    def finish(self, eng_name="sp"):
        e = self.E[eng_name]
        deps = {o: o.count for o in self.E.values() if o is not e}
        for lst in self.dsems.values():
            for d in lst:
                if d.count:
                    deps[d] = d.count
        self._need(e, deps)


def _perm_rope():
    p = np.arange(256)
    half = (p // 8) % 2
    return np.where(half == 0, p + 8, p - 8)


def _rope_tables():
    rows = T // 64
    row_ids = np.repeat(np.arange(rows, dtype=np.float32), 64)
    col_ids = np.tile(np.arange(64, dtype=np.float32), rows)
    freqs = (np.float32(10000.0) ** (-np.arange(8, dtype=np.float32) / np.float32(8))).astype(np.float32)
    ang = np.stack([row_ids[:, None] * freqs, col_ids[:, None] * freqs], axis=1).astype(np.float32)
    cos = np.cos(ang).astype(np.float32)
    sin = np.sin(ang).astype(np.float32)
    p = np.arange(128)
    axis = (p // 16) % 2
    half = (p // 8) % 2
    f = p % 8
    cosT = cos[:, axis, f].T.copy()
    sgn = np.where(half == 0, -1.0, 1.0).astype(np.float32)
    sinT = (sin[:, axis, f].T * sgn[:, None]).astype(np.float32).copy()
    return cosT, sinT


class K:
    pass


def build_nc(stop_after=None, dbg=(), mixers="ABCD", skip_moe=bool(int(_os.environ.get('KSKIPMOE', '0'))), layers=DEPTH):
    nc = bass.Bass("TRN2", target_bir_lowering=False)
    es = contextlib.ExitStack()
    k = K()
    k.nc = nc
    dram = {}

    def din(name, shape, dt=F32):
        dram[name] = nc.dram_tensor(name, list(shape), dt, kind="ExternalInput").ap()
        return dram[name]

    def dout(name, shape, dt=F32):
        if name.startswith("dbg_yTm"):
            dt = BF16
        dram[name] = nc.dram_tensor(name, list(shape), dt, kind="ExternalOutput").ap()
        return dram[name]

    xin = din("xin", [TT, D])
    cT = din("cT", [128, 8, 2])
    w_ada = din("w_ada", [DEPTH, D, 6 * D])
    b_adaT = din("b_adaT", [DEPTH, 128, 48])
    n1g = din("n1g", [DEPTH, 128, 8])
    n2g = din("n2g", [DEPTH, 128, 8])
    fng = din("fng", [128, 8])
    w_in = din("w_in", [DEPTH, D, 3072])
    w_inr = din("w_inr", [DEPTH, D, 512])
    w_out = din("w_out", [DEPTH, D, D])
    router_w = din("router_w", [128, 8, NE])
    router_b = din("router_b", [1, NE])
    moe_w1 = din("moe_w1", [DEPTH, NE, D, DE])
    moe_w3 = din("moe_w3", [DEPTH, NE, D, DE])
    moe_w2 = din("moe_w2", [DEPTH, NE, DE, D])
    lru_cw = din("lru_cw", [DEPTH, 128, 2, 4])
    lru_cb = din("lru_cb", [DEPTH, 128, 2])
    lru_gb = din("lru_gb", [DEPTH, 128, 3, 2, 2])
    lru_wbd = din("lru_wbd", [DEPTH, 128, 2, 2, 2, 128])
    sg_wT = din("sg_wT", [DEPTH, 128, 4, 128])
    sg_bT = din("sg_bT", [DEPTH, 128, 4])
    sg_g = din("sg_g", [DEPTH, 256])
    da_lam = din("da_lam", [DEPTH, 128])
    da_g = din("da_g", [DEPTH, 64])
    m4_d = din("m4", [128, 4])
    cosT_d = din("cosT", [128, T])
    sinT_d = din("sinT", [128, T])
    tri_d = din("tri", [128, 6, 128])
    tokm_d = din("tokm", [128, 2, 6])
    ci_d = din("ci", [128, 2, 4])
    vmask_d = din("vmask", [128, 2, 128])
    hg_g = din("hg_g", [DEPTH, 256])
    hg_lb_d = din("hg_lb", [1, 2 * DEPTH * 256])
    ident_d = din("ident", [128, 128])
    sel_d = din("sel", [NE, NE, 128])
    out_d = dout("out", [T, D])
    dbg_d = {}
    for nm, shp in dbg:
        dbg_d[nm] = dout("dbg_" + nm, shp)

    with es:
        S = Sched(nc, es)

        uid = [0]

        def SBT(ph, name, shape, dt=F32):
            uid[0] += 1
            return ph.enter_context(nc.sbuf_tensor("sb%d_%s" % (uid[0], name), list(shape), dt))

        def PST(ph, name, shape, dt=F32):
            uid[0] += 1
            return ph.enter_context(nc.psum_tensor("ps%d_%s" % (uid[0], name), list(shape), dt))

        def sb(name, shape, dt=F32):
            return SBT(es, name, shape, dt)

        xT = sb("xT", [128, 8, TT])
        hT = sb("hT", [128, 8, TT], BF16)
        ident = sb("ident", [128, 128])
        identb = sb("identb", [128, 128], BF16)
        ones_b = sb("ones_b", [128, 128], BF16)
        scT = sb("scT", [128, 8, 2])
        modTs = [sb("modT%d" % i, [128, 48, 2]) for i in range(DEPTH)]
        gvec = sb("gvec", [128, 3, 8])
        gscs = [sb("gsc%d" % i, [128, 2, 8, 2]) for i in range(DEPTH)]
        scTb = sb("scTb", [128, 8, 2], BF16)
        modT = modTs[0]
        gsc = gscs[0]
        rw = sb("rw", [128, 8, NE])
        rb = sb("rb", [128, NE])

        S.dma("sp", ident[:], ident_d[:, :], w=["ident"])
        S.dma("sp", scT[:], cT[:, :, :], w=["scT"])
        S.dma("sp", rw[:], router_w[:, :, :], w=["rw"])
        S.dma("sp", rb[:], router_b.partition_broadcast(128), w=["rb"])
        S.dma("sp", gvec[:, 2, :], fng[:, :], w=["gvec2"])
        S.op("dve", lambda e: e.tensor_copy(out=identb[:], in_=ident[:]), r=["ident"], w=["identb"])
        S.op("dve", lambda e: e.memset(ones_b[:], 1.0), w=["ones_b"])
        S.op("act", lambda e: e.activation(out=scT[:], in_=scT[:], func=AF.Silu), r=["scT"], w=["scT"])
        S.op("dve", lambda e: e.tensor_copy(out=scTb[:], in_=scT[:]), r=["scT"], w=["scTb"])

        with contextlib.ExitStack() as ph:
            stg = [SBT(ph, "stg%d" % i, [128, D], F32) for i in range(3)]
            tp = [PST(ph, "tp%d" % i, [128, 4, 128], F32) for i in range(2)]
            wa = [SBT(ph, "wa%d" % i, [128, 8, 512], BF16) for i in range(4)]
            mps = [PST(ph, "mp%d" % i, [128, 48, 2], F32) for i in range(DEPTH)]
            badaTs = [SBT(ph, "badaT%d" % i, [128, 48]) for i in range(DEPTH)]
            gvecs = [SBT(ph, "gvecs%d" % i, [128, 2, 8]) for i in range(DEPTH)]

            def gen_xload():
                n = 0
                for t in range(NT):
                    st = stg[t % 3]
                    S.dma("sp", st[:], xin[t * 128:(t + 1) * 128, :], w=[("stg", t % 3)])
                    for half in range(2):
                        p = tp[n % 2]
                        for j in range(4):
                            c = half * 4 + j
                            S.op("pe", lambda e, p=p, j=j, c=c, st=st: e.transpose(out=p[:, j, :], in_=st[:, c * 128:(c + 1) * 128], identity=ident[:]),
                                 r=[("stg", t % 3), "ident"], w=[("tp", n % 2)])
                        if n % 2 == 0:
                            S.op("act", lambda e, p=p, half=half, t=t: e.activation(out=xT[:, half * 4:half * 4 + 4, t * 128:(t + 1) * 128], in_=p[:], func=AF.Copy),
                                 r=[("tp", n % 2)], w=[("xT", t // 4)])
                        else:
                            S.op("dve", lambda e, p=p, half=half, t=t: e.tensor_copy(out=xT[:, half * 4:half * 4 + 4, t * 128:(t + 1) * 128], in_=p[:]),
                                 r=[("tp", n % 2)], w=[("xT", t // 4)])
                        n += 1
                    yield

            def gen_adaln():
                nw = 0
                for l in range(DEPTH):
                    mp = mps[l]
                    S.dma("act", badaTs[l][:], b_adaT[l, :, :], w=[("badaT", l)])
                    S.dma("act", gvecs[l][:, 0, :], n1g[l, :, :], w=[("gvecs", l)])
                    S.dma("act", gvecs[l][:, 1, :], n2g[l, :, :], w=[("gvecs", l)])
                    for jb in range(12):
                        w4 = nw % 4
                        nw += 1
                        wt = wa[w4]
                        S.dma("pool", wt[:], w_ada[l, :, jb * 512:(jb + 1) * 512].rearrange("(k p) n -> p k n", p=128), w=[("wa", w4)])
                        for jj in range(4):
                            j = jb * 4 + jj
                            for kk in range(8):
                                S.op("pe", lambda e, wt=wt, jj=jj, kk=kk, j=j, mp=mp: e.matmul(mp[:, j, :], lhsT=wt[:, kk, jj * 128:(jj + 1) * 128], rhs=scTb[:, kk, :], start=(kk == 0), stop=(kk == 7)),
                                     r=[("wa", w4), "scTb"], w=[("mp", l)])
                        yield
                    S.op("dve", lambda e, l=l, mp=mp: e.tensor_tensor(out=modTs[l][:], in0=mp[:], in1=badaTs[l][:].unsqueeze(2).to_broadcast([128, 48, 2]), op=ALU.add),
                         r=[("mp", l), ("badaT", l)], w=[("modT", l)])
                    for n_, off in ((0, 8), (1, 32)):
                        S.op("dve", lambda e, n_=n_, off=off, l=l: e.tensor_scalar(out=gscs[l][:, n_, :, :], in0=modTs[l][:, off:off + 8, :], scalar1=1.0, scalar2=None, op0=ALU.add),
                             r=[("modT", l)], w=[("gsc", l)])
                        S.op("dve", lambda e, n_=n_, l=l: e.tensor_tensor(out=gscs[l][:, n_, :, :], in0=gscs[l][:, n_, :, :], in1=gvecs[l][:, n_, :].unsqueeze(2).to_broadcast([128, 8, 2]), op=ALU.mult),
                             r=[("gsc", l), ("gvecs", l)], w=[("gsc", l)])
                    yield

            gens = [gen_xload(), gen_adaln()]
            while gens:
                for g in list(gens):
                    try:
                        next(g)
                    except StopIteration:
                        gens.remove(g)
        S.barrier()

        blocks = [(0, 512), (512, 512), (1024, 512), (1536, 512), (2048, 256)]

        def rmsnorm_to_hT(which, nblk, router, gT=None):
            shoff = 0 if which == 0 else 24
            with contextlib.ExitStack() as ph:
                sq = [SBT(ph, "sq%d" % i, [128, 8, 512], BF16) for i in range(2)]
                rs = [SBT(ph, "rs%d" % i, [128, 512], F32) for i in range(2)]
                tmp = [SBT(ph, "ntmp%d" % i, [128, 512], F32) for i in range(3)]
                ssp = [PST(ph, "ssp%d" % i, [128, 512], F32) for i in range(2)]
                if router:
                    hf = [SBT(ph, "hf%d" % i, [128, 8, 512], F32) for i in range(2)]
                    lgp = [PST(ph, "lgp%d" % i, [128, NE], F32) for i in range(2)]
                    gtp = [PST(ph, "gtp%d" % i, [NE, 128], F32) for i in range(2)]
                    rt = SBT(ph, "rt", [128, 2, 96], F32)
                    rsm = SBT(ph, "rsm", [128, 2, 8], F32)
                nt_ = 0
                pend = []
                for bi in range(nblk):
                    t0, n = blocks[bi]
                    s = 0 if t0 < T else 1
                    b2 = bi % 2
                    S.op("act", lambda e, b2=b2, t0=t0, n=n: e.activation(out=sq[b2][:, :, 0:n], in_=xT[:, :, t0:t0 + n], func=AF.Square),
                         r=[("xT", bi)], w=[("sq", b2)])
                    for c in range(8):
                        S.op("pe", lambda e, b2=b2, c=c, n=n: e.matmul(ssp[b2][:, 0:n], lhsT=ones_b[:], rhs=sq[b2][:, c, 0:n], start=(c == 0), stop=(c == 7)),
                             r=[("sq", b2), "ones_b"], w=[("ssp", b2)])
                    S.op("act", lambda e, b2=b2, n=n: e.activation(out=rs[b2][:, 0:n], in_=ssp[b2][:, 0:n], func=AF.Ln, scale=1.0 / D, bias=epsb[:, 0:1]),
                         r=[("ssp", b2), "epsb"], w=[("rs", b2)])
                    S.op("act", lambda e, b2=b2, n=n: e.activation(out=rs[b2][:, 0:n], in_=rs[b2][:, 0:n], func=AF.Exp, scale=-0.5), r=[("rs", b2)], w=[("rs", b2)])
                    for c in range(8):
                        tb = nt_ % 3
                        nt_ += 1
                        S.op("dve", lambda e, tb=tb, c=c, t0=t0, n=n, b2=b2, s=s: e.scalar_tensor_tensor(out=tmp[tb][:, 0:n], in0=xT[:, c, t0:t0 + n], scalar=gsc[:, which, c, s:s + 1], in1=rs[b2][:, 0:n], op0=ALU.mult, op1=ALU.mult),
                             r=[("xT", bi), "gsc", ("rs", b2)], w=[("ntmp", tb)])
                        if not router:
                            S.op("act", lambda e, tb=tb, c=c, t0=t0, n=n, s=s: e.activation(out=hT[:, c, t0:t0 + n], in_=tmp[tb][:, 0:n], func=AF.Identity, bias=modT[:, shoff + c, s:s + 1]),
                                 r=[("ntmp", tb), "modT"], w=[("hT", bi)])
                        else:
                            S.op("act", lambda e, tb=tb, c=c, n=n, s=s, b2=b2: e.activation(out=hf[b2][:, c, 0:n], in_=tmp[tb][:, 0:n], func=AF.Identity, bias=modT[:, shoff + c, s:s + 1]),
                                 r=[("ntmp", tb), "modT"], w=[("hf", b2, c)])
                            S.op("act", lambda e, tb=tb, c=c, t0=t0, n=n, s=s: e.activation(out=hT[:, c, t0:t0 + n], in_=tmp[tb][:, 0:n], func=AF.Identity, bias=modT[:, shoff + c, s:s + 1]),
                                 r=[("ntmp", tb), "modT"], w=[("hT", bi)])
                    if router:
                        for ti in range(n // 128):
                            tk = t0 + ti * 128
                            q2 = (tk // 128) % 2
                            for c in range(8):
                                S.op("pe", lambda e, c=c, ti=ti, b2=b2, q2=q2: e.matmul(lgp[q2][:, :], lhsT=hf[b2][:, c, ti * 128:(ti + 1) * 128], rhs=rw[:, c, :], start=(c == 0), stop=(c == 7)),
                                     r=[("hf", b2, c), "rw"], w=[("lgp", q2)])
                            R = rt[:, q2, :]
                            sm = rsm[:, q2, :]
                            kr = ("rt", q2)
                            lg = R[:, 0:16]
                            ex = R[:, 16:32]
                            pp = R[:, 32:56]
                            gs = R[:, 56:60]
                            gm = R[:, 60:64]
                            me = R[:, 64:80]
                            m2 = R[:, 80:96]
                            S.op("dve", lambda e, q2=q2, lg=lg: e.tensor_tensor(out=lg, in0=lgp[q2][:, :], in1=rb[:], op=ALU.add), r=[("lgp", q2), "rb"], w=[kr])
                            S.op("dve", lambda e, lg=lg, sm=sm: e.tensor_reduce(out=sm[:, 0:1], in_=lg, axis=AX.X, op=ALU.max), r=[kr], w=[kr])
                            S.op("dve", lambda e, sm=sm: e.tensor_scalar(out=sm[:, 1:2], in0=sm[:, 0:1], scalar1=-1.0, scalar2=None, op0=ALU.mult), r=[kr], w=[kr])
                            S.op("act", lambda e, lg=lg, ex=ex, sm=sm: e.activation(out=ex, in_=lg, func=AF.Exp, bias=sm[:, 1:2]), r=[kr], w=[kr])
                            e3 = ex.rearrange("p (g i) -> p g i", i=4)
                            p3 = pp.rearrange("p (g i) -> p g i", i=6)
                            S.op("dve", lambda e, e3=e3, p3=p3: e.tensor_tensor(out=p3[:, :, 0:3], in0=e3[:, :, 0:3], in1=e3[:, :, 1:4], op=ALU.add), r=[kr], w=[kr])
                            S.op("dve", lambda e, e3=e3, p3=p3: e.tensor_tensor(out=p3[:, :, 3:5], in0=e3[:, :, 0:2], in1=e3[:, :, 2:4], op=ALU.add), r=[kr], w=[kr])
                            S.op("dve", lambda e, e3=e3, p3=p3: e.tensor_tensor(out=p3[:, :, 5:6], in0=e3[:, :, 0:1], in1=e3[:, :, 3:4], op=ALU.add), r=[kr], w=[kr])
                            S.op("dve", lambda e, p3=p3, gs=gs: e.tensor_reduce(out=gs, in_=p3, axis=AX.X, op=ALU.max), r=[kr], w=[kr])
                            S.op("dve", lambda e, gs=gs, sm=sm: e.tensor_reduce(out=sm[:, 2:3], in_=gs, axis=AX.X, op=ALU.max), r=[kr], w=[kr])
                            S.op("dve", lambda e, gs=gs, gm=gm, sm=sm: e.tensor_scalar(out=gm, in0=gs, scalar1=sm[:, 2:3], scalar2=None, op0=ALU.is_ge), r=[kr], w=[kr])
                            S.op("dve", lambda e, e3=e3, gm=gm, me=me: e.tensor_tensor(out=me.rearrange("p (g i) -> p g i", i=4), in0=e3, in1=gm.unsqueeze(2).to_broadcast([128, 4, 4]), op=ALU.mult), r=[kr], w=[kr])
                            S.op("dve", lambda e, me=me, sm=sm: e.tensor_reduce(out=sm[:, 3:4], in_=me, axis=AX.X, op=ALU.max), r=[kr], w=[kr])
                            S.op("dve", lambda e, me=me, m2=m2, sm=sm: e.scalar_tensor_tensor(out=m2, in0=me, scalar=sm[:, 3:4], in1=me, op0=ALU.is_lt, op1=ALU.mult), r=[kr], w=[kr])
                            S.op("dve", lambda e, m2=m2, sm=sm: e.tensor_reduce(out=sm[:, 4:5], in_=m2, axis=AX.X, op=ALU.max), r=[kr], w=[kr])
                            S.op("dve", lambda e, me=me, m2=m2, sm=sm: e.scalar_tensor_tensor(out=m2, in0=me, scalar=sm[:, 4:5], in1=me, op0=ALU.is_ge, op1=ALU.mult), r=[kr], w=[kr])
                            S.op("dve", lambda e, sm=sm: e.tensor_tensor(out=sm[:, 5:6], in0=sm[:, 3:4], in1=sm[:, 4:5], op=ALU.add), r=[kr], w=[kr])
                            S.op("dve", lambda e, sm=sm: e.reciprocal(out=sm[:, 6:7], in_=sm[:, 5:6]), r=[kr], w=[kr])
                            S.op("dve", lambda e, m2=m2, sm=sm: e.tensor_scalar(out=m2, in0=m2, scalar1=sm[:, 6:7], scalar2=None, op0=ALU.mult), r=[kr], w=[kr])
                            if pend:
                                pend.pop()()

                            def fin(m2=m2, q2=q2, tk=tk, kr=kr, bi=bi):
                                S.op("pe", lambda e: e.transpose(out=gtp[q2][:, :], in_=m2, identity=ident[:]), r=[kr, "ident"], w=[("gtp", q2)])
                                S.op("act", lambda e: e.activation(out=gT[:, tk:tk + 128], in_=gtp[q2][:, :], func=AF.Copy), r=[("gtp", q2)], w=[("gT", bi)])
                            pend.append(fin)
                while pend:
                    pend.pop()()
            S.barrier()

        def moe(l, nblk, gT):
            with contextlib.ExitStack() as ph:
                sel = SBT(ph, "sel", [NE, NE, 128], BF16)
                g_hi = SBT(ph, "g_hi", [NE, TT], BF16)
                g_lo = SBT(ph, "g_lo", [NE, TT], BF16)
                with contextlib.ExitStack() as tph:
                    sel32 = SBT(tph, "sel32", [NE, NE, 128])
                    g_r = SBT(tph, "g_r", [NE, TT], F32)
                    S.dma("sp", sel32[:], sel_d[:, :, :], w=["sel32"])
                    S.op("dve", lambda e: e.tensor_copy(out=sel[:], in_=sel32[:]), r=["sel32"], w=["sel"])
                    S.op("dve", lambda e: e.tensor_copy(out=g_hi[:], in_=gT[:]), r=[("gT", bi_) for bi_ in range(nblk)], w=["g_hi"])
                    S.op("dve", lambda e: e.tensor_copy(out=g_r[:], in_=g_hi[:]), r=["g_hi"], w=["g_r"])
                    S.op("dve", lambda e: e.tensor_tensor(out=g_lo[:], in0=gT[:], in1=g_r[:], op=ALU.subtract), r=["g_r"] + [("gT", bi_) for bi_ in range(nblk)], w=["g_lo"])
                S.barrier()
                w13 = [SBT(ph, "w13_%d" % i, [128, 8, 2 * DE], BF16) for i in range(2)]
                w2b = [SBT(ph, "w2b_%d" % i, [128, 4, D], BF16) for i in range(2)]
                heT = [SBT(ph, "heT%d" % i, [128, 4, 512], BF16) for i in range(2)]
                Gsb = [SBT(ph, "Gsb%d" % i, [128, 512], F32) for i in range(2)]
                s1 = [SBT(ph, "s1_%d" % i, [128, 512], F32) for i in range(2)]
                t3 = [SBT(ph, "t3_%d" % i, [128, 512], F32) for i in range(2)]
                h1p = [PST(ph, "h1p%d" % i, [128, 512], F32) for i in range(2)]
                h3p = [PST(ph, "h3p%d" % i, [128, 512], F32) for i in range(2)]
                op_ = [PST(ph, "op%d" % i, [128, 512], F32) for i in range(2)]
                gp = PST(ph, "gp", [128, 512], F32)
                cnt = {"nf": 0, "no": 0}

                def load_w(ex):
                    e2 = ex % 2
                    S.dma("pool", w13[e2][:, :, 0:DE], moe_w1[l, ex].rearrange("(k p) f -> p k f", p=128), w=[("w13a", e2)])
                    S.dma("pool", w13[e2][:, :, DE:2 * DE], moe_w3[l, ex].rearrange("(k p) f -> p k f", p=128), w=[("w13b", e2)])
                    S.dma("pool", w2b[e2][:], moe_w2[l, ex].rearrange("(k p) d -> p k d", p=128), w=[("w2b", e2)])

                def up_fc(it, fc):
                    ex, bi, g2 = it
                    e2 = ex % 2
                    t0, n = blocks[bi]
                    f2 = cnt["nf"] % 2
                    cnt["nf"] += 1
                    for kk in range(8):
                        S.op("pe", lambda e, kk=kk: e.matmul(h1p[f2][:, 0:n], lhsT=w13[e2][:, kk, fc * 128:(fc + 1) * 128], rhs=hT[:, kk, t0:t0 + n], start=(kk == 0), stop=(kk == 7)),
                             r=[("w13a", e2), ("hT", bi)], w=[("h1p", f2)])
                    for kk in range(8):
                        S.op("pe", lambda e, kk=kk: e.matmul(h3p[f2][:, 0:n], lhsT=w13[e2][:, kk, DE + fc * 128:DE + (fc + 1) * 128], rhs=hT[:, kk, t0:t0 + n], start=(kk == 0), stop=(kk == 7)),
                             r=[("w13b", e2), ("hT", bi)], w=[("h3p", f2)])
                    S.op("act", lambda e: e.activation(out=s1[f2][:, 0:n], in_=h1p[f2][:, 0:n], func=AF.Silu), r=[("h1p", f2)], w=[("s1", f2)])
                    S.op("dve", lambda e: e.tensor_tensor(out=t3[f2][:, 0:n], in0=h3p[f2][:, 0:n], in1=Gsb[g2][:, 0:n], op=ALU.mult),
                         r=[("h3p", f2), ("Gsb", g2)], w=[("t3", f2)])
                    S.op("pool", lambda e: e.tensor_tensor(out=heT[g2][:, fc, 0:n], in0=s1[f2][:, 0:n], in1=t3[f2][:, 0:n], op=ALU.mult),
                         r=[("s1", f2), ("t3", f2)], w=[("heT", g2, fc)])

                def down_dc(it, dc):
                    ex, bi, g2 = it
                    e2 = ex % 2
                    t0, n = blocks[bi]
                    s = 0 if t0 < T else 1
                    o2 = cnt["no"] % 2
                    cnt["no"] += 1
                    for fc in range(4):
                        S.op("pe", lambda e, fc=fc: e.matmul(op_[o2][:, 0:n], lhsT=w2b[e2][:, fc, dc * 128:(dc + 1) * 128], rhs=heT[g2][:, fc, 0:n], start=(fc == 0), stop=(fc == 3)),
                             r=[("w2b", e2), ("heT", g2, fc)], w=[("op", o2)])
                    S.op("dve", lambda e: e.scalar_tensor_tensor(out=xT[:, dc, t0:t0 + n], in0=op_[o2][:, 0:n], scalar=modT[:, 40 + dc, s:s + 1], in1=xT[:, dc, t0:t0 + n], op0=ALU.mult, op1=ALU.add),
                         r=[("op", o2), "modT", ("xT", bi, dc)], w=[("xT", bi, dc)])

                items = [(ex, bi) for ex in range(NE) for bi in range(nblk)]
                load_w(0)
                prev = None
                for i, (ex, bi) in enumerate(items):
                    t0, n = blocks[bi]
                    g2 = i % 2
                    it = (ex, bi, g2)
                    S.op("pe", lambda e, ex=ex, t0=t0, n=n: e.matmul(gp[:, 0:n], lhsT=sel[:, ex, :], rhs=g_hi[:, t0:t0 + n], start=True, stop=False),
                         r=["sel", "g_hi"], w=["gp"])
                    S.op("pe", lambda e, ex=ex, t0=t0, n=n: e.matmul(gp[:, 0:n], lhsT=sel[:, ex, :], rhs=g_lo[:, t0:t0 + n], start=False, stop=True),
                         r=["sel", "g_lo"], w=["gp"])
                    S.op("act", lambda e, g2=g2, n=n: e.activation(out=Gsb[g2][:, 0:n], in_=gp[:, 0:n], func=AF.Copy), r=["gp"], w=[("Gsb", g2)])
                    for fc in range(4):
                        up_fc(it, fc)
                        if prev is not None:
                            down_dc(prev, 2 * fc)
                            down_dc(prev, 2 * fc + 1)
                    if bi == 0 and ex + 1 < NE:
                        load_w(ex + 1)
                    prev = it
                for dc in range(8):
                    down_dc(prev, dc)
            S.barrier()

        epsb = sb("epsb", [128, 1])
        S.op("dve", lambda e: e.memset(epsb[:], EPS), w=["epsb"])
        c1 = sb("c1", [128, 1])
        S.op("dve", lambda e: e.memset(c1[:], 1.0), w=["c1"])

        def load_win(ph, l, c0, ncols, name):
            wt = SBT(ph, name, [128, 8, ncols], BF16)
            S.dma("pool", wt[:], w_in[l, :, c0:c0 + ncols].rearrange("(k p) n -> p k n", p=128), w=[name])
            return wt

        def proj_out(ph, l, m, yTm, nblk):
            wo = SBT(ph, "wo", [128, 2, D], BF16)
            S.dma("pool", wo[:], w_out[l, m * 256:(m + 1) * 256, :].rearrange("(k p) n -> p k n", p=128), w=["wo"])
            pp = [PST(ph, "pop%d" % i, [128, 512], F32) for i in range(2)]
            no = 0
            for bi in range(nblk):
                t0, n = blocks[bi]
                s = 0 if t0 < T else 1
                for dc in range(8):
                    o2 = no % 2
                    no += 1
                    for j in range(2):
                        S.op("pe", lambda e, o2=o2, j=j, dc=dc, t0=t0, n=n: e.matmul(pp[o2][:, 0:n], lhsT=wo[:, j, dc * 128:(dc + 1) * 128], rhs=yTm[:, j, t0:t0 + n], start=(j == 0), stop=(j == 1)),
                             r=["wo", "yTm"], w=[("pop", o2)])
                    S.op("dve", lambda e, o2=o2, dc=dc, t0=t0, n=n, s=s: e.scalar_tensor_tensor(out=xT[:, dc, t0:t0 + n], in0=pp[o2][:, 0:n], scalar=modT[:, 16 + dc, s:s + 1], in1=xT[:, dc, t0:t0 + n], op0=ALU.mult, op1=ALU.add),
                         r=[("pop", o2), "modT", ("xT", bi, dc)], w=[("xT", bi, dc)])

        def mixer_lru(l, ctx_out):
            nblk = 5 if ctx_out else 4
            CO = T + 3
            with contextlib.ExitStack() as ph:
                yTm = SBT(ph, "yTmA", [128, 2, TT], BF16)
                with contextlib.ExitStack() as ph2:
                    win = load_win(ph2, l, 0, 512, "winA")
                    cw = SBT(ph2, "cw", [128, 2, 4])
                    cb = SBT(ph2, "cb", [128, 2])
                    gb = SBT(ph2, "gb", [128, 3, 2, 2])
                    wbd = SBT(ph2, "wbd", [128, 2, 2, 2, 128], BF16)
                    cl = SBT(ph2, "cl", [128, 2, 2, 2])
                    S.dma("sp", cw[:], lru_cw[l], w=["cw"])
                    S.dma("sp", cb[:], lru_cb[l], w=["cb"])
                    S.dma("sp", gb[:], lru_gb[l], w=["gb"])
                    S.dma("pool", wbd[:], lru_wbd[l], w=["wbd"])
                    S.op("act", lambda e: e.activation(out=cl[:, 0], in_=gb[:, 2], func=AF.Exp, scale=-1.0), r=["gb"], w=["cl"])
                    S.op("dve", lambda e: e.tensor_scalar(out=cl[:, 0], in0=cl[:, 0], scalar1=1.0, scalar2=None, op0=ALU.add), r=["cl"], w=["cl"])
                    S.op("act", lambda e: e.activation(out=cl[:, 0], in_=cl[:, 0], func=AF.Ln), r=["cl"], w=["cl"])
                    S.op("dve", lambda e: e.tensor_scalar(out=cl[:, 1], in0=cl[:, 0], scalar1=-16.0, scalar2=None, op0=ALU.mult), r=["cl"], w=["cl"])
                    S.op("dve", lambda e: e.tensor_scalar(out=cl[:, 0], in0=cl[:, 0], scalar1=-8.0, scalar2=None, op0=ALU.mult), r=["cl"], w=["cl"])
                    xpad = SBT(ph2, "xpad", [128, TT + 6])
                    u = SBT(ph2, "u", [128, TT])
                    ub = SBT(ph2, "ub", [128, TT], BF16)
                    rA = SBT(ph2, "rA", [128, TT])
                    iB = SBT(ph2, "iB", [128, TT])
                    sS = SBT(ph2, "sS", [128, TT])
                    hs = SBT(ph2, "hs", [128, TT])
                    gg = SBT(ph2, "gg", [128, TT])
                    zp = [PST(ph2, "zpA%d" % i, [128, 512], F32) for i in range(4)]
                    S.op("pool", lambda e: e.memset(xpad[:], 0.0), w=["xpad"])
                    nz = 0
                    for cc in range(2):
                        for bi in range(5):
                            t0, n = blocks[bi]
                            xo = (2 + t0) if t0 < T else (CO + 2)
                            z2 = nz % 4
                            nz += 1
                            for kk in range(8):
                                S.op("pe", lambda e, z2=z2, kk=kk, cc=cc, t0=t0, n=n: e.matmul(zp[z2][:, 0:n], lhsT=win[:, kk, cc * 128:(cc + 1) * 128], rhs=hT[:, kk, t0:t0 + n], start=(kk == 0), stop=(kk == 7)),
                                     r=["winA"], w=[("zp", z2)])
                            S.op("act", lambda e, z2=z2, xo=xo, n=n: e.activation(out=xpad[:, xo:xo + n], in_=zp[z2][:, 0:n], func=AF.Copy), r=[("zp", z2)], w=["xpad"])
                            if bi < nblk:
                                z2 = nz % 4
                                nz += 1
                                for kk in range(8):
                                    S.op("pe", lambda e, z2=z2, kk=kk, cc=cc, t0=t0, n=n: e.matmul(zp[z2][:, 0:n], lhsT=win[:, kk, 256 + cc * 128:256 + (cc + 1) * 128], rhs=hT[:, kk, t0:t0 + n], start=(kk == 0), stop=(kk == 7)),
                                         r=["winA"], w=[("zp", z2)])
                                S.op("act", lambda e, z2=z2, t0=t0, n=n: e.activation(out=gg[:, t0:t0 + n], in_=zp[z2][:, 0:n], func=AF.Gelu_apprx_tanh), r=[("zp", z2)], w=["gg"])
                        for (xo, uo, L) in ((0, 0, T), (CO, T, TC)):
                            S.op("dve", lambda e, xo=xo, uo=uo, L=L, cc=cc: e.tensor_scalar(out=u[:, uo:uo + L], in0=xpad[:, xo:xo + L], scalar1=cw[:, cc, 0:1], scalar2=cb[:, cc:cc + 1], op0=ALU.mult, op1=ALU.add),
                                 r=["xpad", "cw", "cb"], w=["u"])
                            for j in range(1, 4):
                                S.op("dve", lambda e, xo=xo, uo=uo, L=L, cc=cc, j=j: e.scalar_tensor_tensor(out=u[:, uo:uo + L], in0=xpad[:, xo + j:xo + j + L], scalar=cw[:, cc, j:j + 1], in1=u[:, uo:uo + L], op0=ALU.mult, op1=ALU.add),
                                     r=["xpad", "cw", "u"], w=["u"])
                        S.op("act", lambda e: e.activation(out=ub[:], in_=u[:], func=AF.Copy), r=["u"], w=["ub"])
                        if KCUT == 1:
                            continue
                        for d in range(2):
                            for bi in range(5):
                                t0, n = blocks[bi]
                                for gi, dst in ((0, rA), (1, iB)):
                                    z2 = nz % 4
                                    nz += 1
                                    S.op("pe", lambda e, z2=z2, gi=gi, d=d, cc=cc, t0=t0, n=n: e.matmul(zp[z2][:, 0:n], lhsT=wbd[:, gi, d, cc, :], rhs=ub[:, t0:t0 + n], start=True, stop=True),
                                         r=["wbd", "ub"], w=[("zp", z2)])
                                    S.op("act", lambda e, z2=z2, gi=gi, d=d, cc=cc, t0=t0, n=n, dst=dst: e.activation(out=dst[:, t0:t0 + n], in_=zp[z2][:, 0:n], func=AF.Sigmoid, bias=gb[:, gi, d, cc:cc + 1]),
                                         r=[("zp", z2), "gb"], w=["rA" if gi == 0 else "iB"])
                            if KCUT == 2:
                                continue
                            S.op("act", lambda e, d=d, cc=cc: e.activation(out=sS[:], in_=rA[:], func=AF.Exp, scale=cl[:, 1, d, cc:cc + 1]), r=["rA", "cl", "hs"], w=["sS"])
                            S.op("dve", lambda e: e.tensor_scalar(out=sS[:], in0=sS[:], scalar1=1.0, scalar2=-1.0, op0=ALU.min, op1=ALU.mult), r=["sS"], w=["sS"])
                            S.op("act", lambda e: e.activation(out=sS[:], in_=sS[:], func=AF.Sqrt, bias=c1[:, 0:1]), r=["sS", "c1"], w=["sS"])
                            S.op("act", lambda e, d=d, cc=cc: e.activation(out=rA[:], in_=rA[:], func=AF.Exp, scale=cl[:, 0, d, cc:cc + 1]), r=["rA", "cl"], w=["rA"])
                            S.op("dve", lambda e: e.tensor_tensor(out=iB[:], in0=iB[:], in1=sS[:], op=ALU.mult), r=["iB", "sS"], w=["iB"])
                            S.op("dve", lambda e: e.tensor_tensor(out=iB[:], in0=iB[:], in1=u[:], op=ALU.mult), r=["iB", "u"], w=["iB"])
                            if KCUT == 3:
                                continue
                            if d == 0:
                                S.op("dve", lambda e: e.tensor_tensor_scan(out=hs[:, T:TT], data0=rA[:, T:TT], data1=iB[:, T:TT], initial=0.0, op0=ALU.mult, op1=ALU.add),
                                     r=["rA", "iB"], w=["hs"])
                                S.op("dve", lambda e: e.tensor_tensor_scan(out=hs[:, 0:T], data0=rA[:, 0:T], data1=iB[:, 0:T], initial=hs[:, TT - 1:TT], op0=ALU.mult, op1=ALU.add),
                                     r=["rA", "iB", "hs"], w=["hs"])
                            elif KCUT != 4:
                                S.op("dve", lambda e: e.tensor_tensor_scan(out=sS[:, T:TT][:, ::-1], data0=rA[:, T:TT][:, ::-1], data1=iB[:, T:TT][:, ::-1], initial=0.0, op0=ALU.mult, op1=ALU.add),
                                     r=["rA", "iB", "sS"], w=["sS"])
                                S.op("dve", lambda e: e.tensor_tensor_scan(out=sS[:, 0:T][:, ::-1], data0=rA[:, 0:T][:, ::-1], data1=iB[:, 0:T][:, ::-1], initial=sS[:, T:T + 1], op0=ALU.mult, op1=ALU.add),
                                     r=["rA", "iB", "sS"], w=["sS"])
                                S.op("dve", lambda e: e.tensor_tensor(out=hs[:], in0=hs[:], in1=sS[:], op=ALU.add), r=["hs", "sS"], w=["hs"])
                        ny = TT if ctx_out else T
                        if KCUT in (4, 5):
                            continue
                        S.op("dve", lambda e, cc=cc, ny=ny: e.tensor_tensor(out=yTm[:, cc, 0:ny], in0=hs[:, 0:ny], in1=gg[:, 0:ny], op=ALU.mult), r=["hs", "gg"], w=["yTm"])
                S.barrier()
                dump("yTmA%d" % l, yTm[:])
                with contextlib.ExitStack() as ph3:
                    proj_out(ph3, l, 0, yTm, nblk)
            S.barrier()

        def mixer_sg(l, ctx_out):
            nblk = 5 if ctx_out else 4
            ntile = NT if ctx_out else T // 128
            with contextlib.ExitStack() as ph:
                yTm = SBT(ph, "yTmC", [128, 2, TT], BF16)
                with contextlib.ExitStack() as ph2:
                    win = load_win(ph2, l, 1280, 512, "winC")
                    wsp = SBT(ph2, "wsp", [128, 4, 128], BF16)
                    bsp = SBT(ph2, "bsp", [128, 4])
                    gbc = SBT(ph2, "gbc", [128, 256])
                    S.dma("pool", wsp[:], sg_wT[l], w=["wsp"])
                    S.dma("sp", bsp[:], sg_bT[l], w=["bsp"])
                    S.dma("sp", gbc[:], sg_g[l:l + 1, :].partition_broadcast(128), w=["gbc"])
                    zp = [PST(ph2, "zpC%d" % i, [128, 512], F32) for i in range(3)]
                    vmp = [PST(ph2, "vmp%d" % i, [128, 256], F32) for i in range(3)]
                    tpp = [PST(ph2, "tppC%d" % i, [128, 2, 128], BF16) for i in range(2)]
                    g = [SBT(ph2, "gC%d" % i, [128, 512]) for i in range(3)]
                    vb = [SBT(ph2, "vbC%d" % i, [128, 256], BF16) for i in range(3)]
                    ys = [SBT(ph2, "ysC%d" % i, [128, 256], BF16) for i in range(3)]
                    sm = SBT(ph2, "smC", [128, 3, 4])
                    junk = [SBT(ph2, "junkC%d" % i, [128, 256]) for i in range(3)]
                    def stA(t):
                        t2 = t % 3
                        cs = slice(t * 128, (t + 1) * 128)
                        for kk in range(8):
                            S.op("pe", lambda e, kk=kk: e.matmul(zp[t2][:, :], lhsT=hT[:, kk, cs], rhs=win[:, kk, :], start=(kk == 0), stop=(kk == 7)),
                                 r=["winC"], w=[("zp", t2)])
                        S.op("act", lambda e: e.activation(out=g[t2][:], in_=zp[t2][:, :], func=AF.Gelu_apprx_tanh), r=[("zp", t2)], w=[("g", t2)])
                        S.op("act", lambda e: e.activation(out=junk[t2][:], in_=g[t2][:, 256:512], func=AF.Square, accum_out=sm[:, t2, 0:1]), r=[("g", t2)], w=[("junk", t2), ("sm", t2)])
                        S.op("act", lambda e: e.activation(out=sm[:, t2, 1:2], in_=sm[:, t2, 0:1], func=AF.Sqrt, scale=1.0 / 256, bias=epsb[:, 0:1]), r=[("sm", t2), "epsb"], w=[("sm", t2)])
                        S.op("dve", lambda e: e.reciprocal(out=sm[:, t2, 2:3], in_=sm[:, t2, 1:2]), r=[("sm", t2)], w=[("sm", t2)])
                        S.op("dve", lambda e: e.scalar_tensor_tensor(out=vb[t2][:], in0=g[t2][:, 256:512], scalar=sm[:, t2, 2:3], in1=gbc[:], op0=ALU.mult, op1=ALU.mult),
                             r=[("g", t2), ("sm", t2), "gbc"], w=[("vb", t2)])

                    def stB(t):
                        t2 = t % 3
                        for h in range(4):
                            S.op("pe", lambda e, h=h: e.matmul(vmp[t2][:, h * 64:(h + 1) * 64], lhsT=wsp[:, h, :], rhs=vb[t2][:, h * 64:(h + 1) * 64], start=True, stop=True),
                                 r=["wsp", ("vb", t2)], w=[("vmp", t2)])
                        for h in range(4):
                            S.op("dve", lambda e, h=h: e.scalar_tensor_tensor(out=ys[t2][:, h * 64:(h + 1) * 64], in0=vmp[t2][:, h * 64:(h + 1) * 64], scalar=bsp[:, h:h + 1], in1=g[t2][:, h * 64:(h + 1) * 64], op0=ALU.add, op1=ALU.mult),
                                 r=[("vmp", t2), "bsp", ("g", t2)], w=[("ys", t2)])

                    def stC(t):
                        t2 = t % 3
                        tq = t % 2
                        cs = slice(t * 128, (t + 1) * 128)
                        for j in range(2):
                            S.op("pe", lambda e, j=j: e.transpose(out=tpp[tq][:, j, :], in_=ys[t2][:, j * 128:(j + 1) * 128], identity=identb[:]),
                                 r=[("ys", t2), "identb"], w=[("tpp", tq)])
                        S.op("act", lambda e: e.activation(out=yTm[:, :, cs], in_=tpp[tq][:], func=AF.Copy), r=[("tpp", tq)], w=["yTm"])

                    for t in range(ntile + 2):
                        if t < ntile:
                            stA(t)
                        if 0 <= t - 1 < ntile:
                            stB(t - 1)
                        if 0 <= t - 2 < ntile:
                            stC(t - 2)
                S.barrier()
                dump("yTmC%d" % l, yTm[:])
                with contextlib.ExitStack() as ph3:
                    proj_out(ph3, l, 2, yTm, nblk)
            S.barrier()

        def mixer_attn(l, ctx_out):
            nblk = 5 if ctx_out else 4
            nqt = NT if ctx_out else T // 128
            lam_init = 0.8 - 0.6 * math.exp(-0.3 * l)
            SC = 32.0 ** -0.5
            with contextlib.ExitStack() as ph:
                yTm = SBT(ph, "yTmB", [128, 2, TT], BF16)
                with contextlib.ExitStack() as ph1:
                    qT = SBT(ph1, "qT", [128, 2, TT], BF16)
                    kTm = [SBT(ph1, "kTm%d" % j, [128, 2, TT], BF16) for j in range(4)]
                    V = SBT(ph1, "V", [128, NT, 4, 65], BF16)
                    lamv = SBT(ph1, "lamv", [128, 4])
                    gsub = SBT(ph1, "gsub", [128, 64])
                    dl = SBT(ph1, "dl", [128, 128])
                    pr = SBT(ph1, "pr", [128, 64])
                    m4 = SBT(ph1, "m4", [128, 4])
                    S.dma("sp", dl[:], da_lam[l:l + 1, :].partition_broadcast(128), w=["dl"])
                    S.dma("sp", gsub[:], da_g[l:l + 1, :].partition_broadcast(128), w=["gsub"])
                    S.dma("sp", m4[:], m4_d[:, :], w=["m4"])
                    S.op("dve", lambda e: e.tensor_scalar(out=gsub[:], in0=gsub[:], scalar1=1.0 - lam_init, scalar2=None, op0=ALU.mult), r=["gsub"], w=["gsub"])
                    S.op("dve", lambda e: e.tensor_tensor(out=pr[:, 0:32], in0=dl[:, 0:32], in1=dl[:, 32:64], op=ALU.mult), r=["dl"], w=["pr"])
                    S.op("dve", lambda e: e.tensor_tensor(out=pr[:, 32:64], in0=dl[:, 64:96], in1=dl[:, 96:128], op=ALU.mult), r=["dl", "pr"], w=["pr"])
                    S.op("dve", lambda e: e.tensor_reduce(out=lamv[:, 0:2], in_=pr[:].rearrange("p (a b) -> p a b", b=32), axis=AX.X, op=ALU.add), r=["pr"], w=["lamv"])
                    S.op("act", lambda e: e.activation(out=lamv[:, 0:2], in_=lamv[:, 0:2], func=AF.Exp), r=["lamv"], w=["lamv"])
                    S.op("dve", lambda e: e.tensor_tensor(out=lamv[:, 2:3], in0=lamv[:, 0:1], in1=lamv[:, 1:2], op=ALU.subtract), r=["lamv"], w=["lamv"])
                    S.op("dve", lambda e: e.tensor_scalar(out=lamv[:, 3:4], in0=lamv[:, 2:3], scalar1=lam_init, scalar2=None, op0=ALU.add), r=["lamv"], w=["lamv"])
                    with contextlib.ExitStack() as ph2:
                        win = load_win(ph2, l, 512, 512, "winB")
                        winR = SBT(ph2, "winR", [128, 8, 512], BF16)
                        S.dma("pool", winR[:], w_inr[l].rearrange("(k p) n -> p k n", p=128), w=["winR"])
                        cs_t = [SBT(ph2, "cs%d" % i, [128, 2, 512]) for i in range(2)]
                        zp = [PST(ph2, "zpB%d" % i, [128, 512], F32) for i in range(4)]
                        t1 = [SBT(ph2, "t1_%d" % i, [128, 512]) for i in range(2)]
                        t2 = [SBT(ph2, "t2_%d" % i, [128, 512]) for i in range(2)]
                        S.op("pool", lambda e: e.memset(V[:].rearrange("p t h e -> p (t h) e")[:, :, 64:65], 1.0), w=["Vones"])
                        nz = 0
                        ni = 0
                        for c in range(2):
                            for which in range(2):
                                for bi in range(5):
                                    t0, n = blocks[bi]
                                    lat = t0 < T
                                    if which == 0 and not lat and not ctx_out:
                                        continue
                                    za = nz % 4
                                    nz += 1
                                    for kk in range(8):
                                        S.op("pe", lambda e, za=za, kk=kk, c=c, which=which, t0=t0, n=n: e.matmul(zp[za][:, 0:n], lhsT=win[:, kk, which * 256 + c * 128:which * 256 + (c + 1) * 128], rhs=hT[:, kk, t0:t0 + n], start=(kk == 0), stop=(kk == 7)),
                                             r=["winB"], w=[("zp", za)])
                                    if lat:
                                        zb = nz % 4
                                        nz += 1
                                        i2 = ni % 2
                                        ni += 1
                                        S.dma("sp", cs_t[i2][:, 0, :], cosT_d[:, t0:t0 + n], w=[("cs", i2)])
                                        S.dma("sp", cs_t[i2][:, 1, :], sinT_d[:, t0:t0 + n], w=[("cs", i2)])
                                        for kk in range(8):
                                            S.op("pe", lambda e, zb=zb, kk=kk, c=c, which=which, t0=t0, n=n: e.matmul(zp[zb][:, 0:n], lhsT=winR[:, kk, which * 256 + c * 128:which * 256 + (c + 1) * 128], rhs=hT[:, kk, t0:t0 + n], start=(kk == 0), stop=(kk == 7)),
                                                 r=["winR"], w=[("zp", zb)])
                                        S.op("dve", lambda e, za=za, i2=i2: e.tensor_tensor(out=t1[i2][:], in0=zp[za][:, :], in1=cs_t[i2][:, 0, :], op=ALU.mult), r=[("zp", za), ("cs", i2)], w=[("t1", i2)])
                                        S.op("dve", lambda e, zb=zb, i2=i2: e.tensor_tensor(out=t2[i2][:], in0=zp[zb][:, :], in1=cs_t[i2][:, 1, :], op=ALU.mult), r=[("zp", zb), ("cs", i2)], w=[("t2", i2)])
                                        if which == 0:
                                            S.op("dve", lambda e, i2=i2, c=c, t0=t0, n=n: e.tensor_tensor(out=qT[:, c, t0:t0 + n], in0=t1[i2][:], in1=t2[i2][:], op=ALU.add), r=[("t1", i2), ("t2", i2)], w=["qT"])
                                        else:
                                            S.op("dve", lambda e, i2=i2: e.tensor_tensor(out=t1[i2][:], in0=t1[i2][:], in1=t2[i2][:], op=ALU.add), r=[("t1", i2), ("t2", i2)], w=[("t1", i2)])
                                            for j in range(4):
                                                en = ("dve", "act", "pool", "act")[j]
                                                if en != "act":
                                                    S.op(en, lambda e, i2=i2, j=j, c=c, t0=t0, n=n: e.tensor_scalar(out=kTm[j][:, c, t0:t0 + n], in0=t1[i2][:], scalar1=m4[:, j:j + 1], scalar2=None, op0=ALU.mult), r=[("t1", i2), "m4"], w=["kTm"])
                                                else:
                                                    S.op("act", lambda e, i2=i2, j=j, c=c, t0=t0, n=n: e.activation(out=kTm[j][:, c, t0:t0 + n], in_=t1[i2][:], func=AF.Copy, scale=m4[:, j:j + 1]), r=[("t1", i2), "m4"], w=["kTm"])
                                    else:
                                        if which == 0:
                                            S.op("act", lambda e, za=za, c=c, t0=t0, n=n: e.activation(out=qT[:, c, t0:t0 + n], in_=zp[za][:, 0:n], func=AF.Copy), r=[("zp", za)], w=["qT"])
                                        else:
                                            for j in range(4):
                                                S.op("dve", lambda e, za=za, j=j, c=c, t0=t0, n=n: e.tensor_scalar(out=kTm[j][:, c, t0:t0 + n], in0=zp[za][:, 0:n], scalar1=m4[:, j:j + 1], scalar2=None, op0=ALU.mult), r=[("zp", za), "m4"], w=["kTm"])
                    S.barrier()
                    with contextlib.ExitStack() as ph2:
                        win = load_win(ph2, l, 1024, 256, "winBv")
                        zp = [PST(ph2, "zpBv%d" % i, [128, 512], F32) for i in range(4)]
                        nz = 0
                        for t in range(NT):
                            za = nz % 4
                            nz += 1
                            for kk in range(8):
                                S.op("pe", lambda e, za=za, kk=kk, t=t: e.matmul(zp[za][:, 0:256], lhsT=hT[:, kk, t * 128:(t + 1) * 128], rhs=win[:, kk, 0:256], start=(kk == 0), stop=(kk == 7)),
                                     r=["winBv"], w=[("zp", za)])
                            S.op("act", lambda e, za=za, t=t: e.activation(out=V[:, t, :, 0:64], in_=zp[za][:, 0:256].rearrange("p (h e) -> p h e", e=64), func=AF.Copy), r=[("zp", za)], w=["V"])
                    S.barrier()
                    with contextlib.ExitStack() as ph3:
                        ytm = SBT(ph3, "ytm", [128, NT, 256], BF16)
                        stp = [PST(ph3, "stp%d" % i, [128, 512], F32) for i in range(3)]
                        ob = [PST(ph3, "ob%d" % i, [128, 4, 65], F32) for i in range(4)]
                        pt = [SBT(ph3, "pt%d" % i, [128, 512], BF16) for i in range(3)]
                        ta = [SBT(ph3, "ta%d" % i, [128, 4, 64]) for i in range(2)]
                        tb = [SBT(ph3, "tb%d" % i, [128, 4, 64]) for i in range(2)]
                        rl = SBT(ph3, "rl", [128, 2, 4, 4])
                        st = {"step": 0, "it": 0}

                        def attend(h, q0, nq, kts):
                            c = h // 2
                            jb = (h % 2) * 2
                            nqs = nq // 128
                            it = st["it"] % 2
                            st["it"] += 1
                            O = [ob[it * 2 + n] for n in range(2)]
                            first = [True, True]
                            steps = [(ki, kt, n) for ki, kt in enumerate(kts) for n in range(2)]
                            bufs = {}

                            def emit_st(i):
                                ki, kt, n = steps[i]
                                i3 = st["step"] % 3
                                st["step"] += 1
                                bufs[i] = i3
                                S.op("pe", lambda e, i3=i3, n=n, kt=kt: e.matmul(stp[i3][:, 0:nq], lhsT=kTm[jb + n][:, c, kt * 128:(kt + 1) * 128], rhs=qT[:, c, q0:q0 + nq], start=True, stop=True),
                                     r=[], w=[("stp", i3)])
                                S.op("act", lambda e, i3=i3: e.activation(out=pt[i3][:, 0:nq], in_=stp[i3][:, 0:nq], func=AF.Exp, scale=SC), r=[("stp", i3)], w=[("pt", i3)])

                            def emit_pv(i):
                                ki, kt, n = steps[i]
                                i3 = bufs[i]
                                for qs in range(nqs):
                                    S.op("pe", lambda e, i3=i3, n=n, kt=kt, qs=qs, fs=first[n], ls=(ki == len(kts) - 1 and qs == nqs - 1): e.matmul(O[n][:, qs, :], lhsT=pt[i3][:, qs * 128:(qs + 1) * 128], rhs=V[:, kt, h, :], start=fs, stop=ls, skip_group_check=True),
                                         r=[("pt", i3)], w=[("O", it, n)])
                                    first[n] = False

                            LOOK = 2
                            for i in range(len(steps)):
                                emit_st(i)
                                if i >= LOOK:
                                    emit_pv(i - LOOK)
                            for i in range(max(0, len(steps) - LOOK), len(steps)):
                                emit_pv(i)
                            A_ = ta[it]
                            B_ = tb[it]
                            R_ = rl[:, it]
                            kr = ("rl", it)
                            for n in range(2):
                                S.op("dve", lambda e, n=n: e.reciprocal(out=R_[:, n, 0:nqs], in_=O[n][:, 0:nqs, 64]), r=[("O", it, n)], w=[kr])
                            S.op("dve", lambda e: e.tensor_scalar(out=R_[:, 1, 0:nqs], in0=R_[:, 1, 0:nqs], scalar1=lamv[:, 3:4], scalar2=None, op0=ALU.mult), r=[kr, "lamv"], w=[kr])
                            S.op("dve", lambda e: e.tensor_tensor(out=A_[:, 0:nqs, :], in0=O[1][:, 0:nqs, 0:64], in1=R_[:, 1, 0:nqs].unsqueeze(2).to_broadcast([128, nqs, 64]), op=ALU.mult), r=[("O", it, 1), kr], w=[("ta", it)])
                            S.op("dve", lambda e: e.tensor_tensor(out=B_[:, 0:nqs, :], in0=O[0][:, 0:nqs, 0:64], in1=R_[:, 0, 0:nqs].unsqueeze(2).to_broadcast([128, nqs, 64]), op=ALU.mult), r=[("O", it, 0), kr], w=[("tb", it)])
                            S.op("pool", lambda e: e.tensor_tensor(out=B_[:, 0:nqs, :], in0=B_[:, 0:nqs, :], in1=A_[:, 0:nqs, :], op=ALU.subtract), r=[("ta", it), ("tb", it)], w=[("tb", it)])
                            S.op("pool", lambda e: e.tensor_tensor(out=A_[:, 0:nqs, :], in0=B_[:, 0:nqs, :], in1=B_[:, 0:nqs, :], op=ALU.mult), r=[("tb", it)], w=[("ta", it)])
                            S.op("dve", lambda e: e.tensor_reduce(out=R_[:, 2, 0:nqs], in_=A_[:, 0:nqs, :], axis=AX.X, op=ALU.add), r=[("ta", it)], w=[kr])
                            S.op("act", lambda e: e.activation(out=R_[:, 3, 0:nqs], in_=R_[:, 2, 0:nqs], func=AF.Ln, scale=1.0 / 64, bias=epsb[:, 0:1]), r=[kr, "epsb"], w=[kr])
                            S.op("act", lambda e: e.activation(out=R_[:, 3, 0:nqs], in_=R_[:, 3, 0:nqs], func=AF.Exp, scale=-0.5), r=[kr], w=[kr])
                            S.op("dve", lambda e: e.tensor_tensor(out=A_[:, 0:nqs, :], in0=B_[:, 0:nqs, :], in1=R_[:, 3, 0:nqs].unsqueeze(2).to_broadcast([128, nqs, 64]), op=ALU.mult), r=[("tb", it), kr], w=[("ta", it)])
                            qt0 = q0 // 128
                            S.op("pool", lambda e: e.tensor_tensor(out=ytm[:, qt0:qt0 + nqs, h * 64:(h + 1) * 64], in0=A_[:, 0:nqs, :], in1=gsub[:].unsqueeze(1).to_broadcast([128, nqs, 64]), op=ALU.mult), r=[("ta", it), "gsub"], w=["ytm"])

                        for h in range(4):
                            for qb in range(4):
                                attend(h, qb * 512, 512, list(range(NT)))
                        if ctx_out:
                            for h in range(4):
                                attend(h, T, TC, [16, 17])
                        S.barrier()
                        with contextlib.ExitStack() as ph4:
                            tpp = [PST(ph4, "tppB%d" % i, [128, 2, 128], BF16) for i in range(1)]
                            for t in range(nqt):
                                t2_ = 0
                                for j in range(2):
                                    S.op("pe", lambda e, t=t, j=j: e.transpose(out=tpp[t2_][:, j, :], in_=ytm[:, t, j * 128:(j + 1) * 128], identity=identb[:]),
                                         r=["identb"], w=[("tpp", t2_)])
                                if t % 2 == 0:
                                    S.op("act", lambda e, t=t: e.activation(out=yTm[:, :, t * 128:(t + 1) * 128], in_=tpp[t2_][:], func=AF.Copy), r=[("tpp", t2_)], w=["yTm"])
                                else:
                                    S.op("dve", lambda e, t=t: e.tensor_copy(out=yTm[:, :, t * 128:(t + 1) * 128], in_=tpp[t2_][:]), r=[("tpp", t2_)], w=["yTm"])
                S.barrier()
                dump("yTmB%d" % l, yTm[:])
                with contextlib.ExitStack() as ph5:
                    proj_out(ph5, l, 1, yTm, nblk)
            S.barrier()

        def mixer_hgrn(l, ctx_out):
            nblk = 5 if ctx_out else 4
            nqt = NT if ctx_out else T // 128
            with contextlib.ExitStack() as ph:
                yTm = SBT(ph, "yTmD", [128, 2, TT], BF16)
                with contextlib.ExitStack() as ph1:
                    win = load_win(ph1, l, 1792, 1280, "winD")
                    osum = SBT(ph1, "osum", [128, nqt, 256])
                    sgall = SBT(ph1, "sgall", [128, nqt, 256], BF16)
                    tri = SBT(ph1, "tri", [128, 6, 128])
                    tokm = SBT(ph1, "tokm", [128, 2, 6])
                    S.dma("sp", tokm[:], tokm_d[:, :, :], w=["tokm"])
                    CI = SBT(ph1, "CI", [128, 2, 4])
                    vmf = osum[:, 0, :].rearrange("p (a b) -> p a b", a=2)
                    vm8 = SBT(ph1, "vm8", [128, 2, 4, 128], mybir.dt.uint8)
                    S.dma("sp", vmf, vmask_d[:, :, :], w=["vmf", ("osum", 0)])
                    for d in range(2):
                        S.op("dve", lambda e, d=d: e.tensor_copy(out=vm8[:, d], in_=vmf[:, d, :].unsqueeze(1).to_broadcast([128, 4, 128])), r=["vmf", ("osum", 0)], w=["vm8"])
                    gnb = SBT(ph1, "gnb", [128, 256])
                    S.dma("sp", tri[:], tri_d[:, :, :], w=["tri"])
                    S.dma("sp", CI[:], ci_d[:, :, :], w=["CI"])
                    S.dma("sp", gnb[:], hg_g[l:l + 1, :].partition_broadcast(128), w=["gnb"])
                    if l > 0:
                        lbb = SBT(ph1, "lbb", [128, 2, 2, 256])
                        with contextlib.ExitStack() as ph0:
                            lbr = SBT(ph0, "lbr", [128, 2, 2, 256])
                            S.dma("sp", lbr[:].rearrange("p a b w -> p (a b w)"), hg_lb_d[0:1, :].partition_broadcast(128), w=["lbr"])
                            S.op("dve", lambda e: e.tensor_tensor(out=lbb[:, 0], in0=lbr[:, :, 0, :], in1=lbr[:, :, 1, :], op=ALU.subtract), r=["lbr"], w=["lbb"])
                            S.op("act", lambda e: e.activation(out=lbb[:, 0], in_=lbb[:, 0], func=AF.Exp), r=["lbb"], w=["lbb"])
                            S.op("dve", lambda e: e.tensor_scalar(out=lbb[:, 0], in0=lbb[:, 0], scalar1=1.0, scalar2=None, op0=ALU.add), r=["lbb"], w=["lbb"])
                            S.op("dve", lambda e: e.reciprocal(out=lbb[:, 0], in_=lbb[:, 0]), r=["lbb"], w=["lbb"])
                            S.op("dve", lambda e: e.tensor_scalar(out=lbb[:, 1], in0=lbb[:, 0], scalar1=-1.0, scalar2=1.0, op0=ALU.mult, op1=ALU.add), r=["lbb"], w=["lbb"])
                            S.barrier()
                    zA = PST(ph1, "zA", [128, 512], F32)
                    zB = PST(ph1, "zB", [128, 512], F32)
                    bc = PST(ph1, "bc", [128, 2, 256], F32)
                    misc = PST(ph1, "misc", [128, 512], F32)
                    tp = PST(ph1, "tpD", [128, 8, 128], BF16)
                    attp = PST(ph1, "attp", [128, 4, 128], F32)
                    op2 = [PST(ph1, "oD%d" % i, [128, 256], F32) for i in range(2)]
                    decp = misc[:, 0:8].rearrange("p (c a) -> p c a", a=4)
                    Pps = misc[:, 128:384].rearrange("p (c h v) -> p c h v", h=2, v=64)
                    S32 = [[SBT(ph1, "S32_%d%d" % (d, k_), [128, 2, 64]) for k_ in range(2)] for d in range(2)]
                    scur = [0, 0]
                    Sbf = [SBT(ph1, "Sbf_%d" % d, [128, 2, 2, 2, 64], BF16) for d in range(2)]
                    for t in range(nqt):
                        S.op("pool", lambda e, t=t: e.memset(osum[:, t, :], 0.0), w=[("osum", t)])
                    for d in range(2):
                        for k_ in range(2):
                            S.op("pool", lambda e, d=d, k_=k_: e.memset(S32[d][k_][:], 0.0), w=[("S32", d, k_)])
                        S.op("pool", lambda e, d=d: e.memset(Sbf[d][:], 0.0), w=[("Sbf", d, xy, hh) for xy in range(2) for hh in range(2)])
                    W = {}
                    for d in range(2):
                        for b in range(1):
                            W[d, b] = dict(
                                kk=SBT(ph1, "kk_%d%d" % (d, b), [128, 256]),
                                ex=SBT(ph1, "ex_%d%d" % (d, b), [128, 3, 256]), dec=SBT(ph1, "dec_%d%d" % (d, b), [128, 2, 4]),
                                q32=SBT(ph1, "q32_%d%d" % (d, b), [128, 256]),
                                qt=SBT(ph1, "qt_%d%d" % (d, b), [128, 4, 256], BF16), kh=SBT(ph1, "kh_%d%d" % (d, b), [128, 2, 256], BF16), kTp=SBT(ph1, "kTp_%d%d" % (d, b), [128, 2, 4, 128], BF16),
                                v=[SBT(ph1, "v_%d%d_%d" % (d, b, k_), [128, 256], BF16) for k_ in range(2)], qkT=SBT(ph1, "qkT_%d%d" % (d, b), [128, 4, 128], BF16),
                                attm=SBT(ph1, "attm_%d%d" % (d, b), [128, 4, 128], BF16),
                            )
                            W[d, b]["f"] = W[d, b]["ex"][:, 0, :]
                            W[d, b]["lf"] = W[d, b]["ex"][:, 2, :]
                            if d == 0:
                                W[d, b]["gtmp"] = SBT(ph1, "gtmp_%d%d" % (d, b), [128, 256])

                    for key_ in W:
                        S.op("pool", lambda e, key_=key_: e.memset(W[key_]["attm"][:], 0.0), w=[("attm",) + key_])
                        S.op("pool", lambda e, key_=key_: e.memset(W[key_]["kTp"][:], 0.0), w=[("kTp",) + key_])

                    def prep(t, d, b, need_out, it):
                        w = W[d, b]
                        vv = w["v"][it % 2]
                        kv = ("v", d, it % 2)
                        kf = ("ex", d, 0)
                        klf = ("ex", d, 2)
                        kkk = ("kk", d, b)
                        cs = slice(t * 128, (t + 1) * 128)
                        groups = [(zA, 0, 0), (zA, 256, 256 + d * 256), (zB, 0, 768)]
                        if d == 0 and need_out:
                            groups.append((zB, 256, 1024))
                        for (zt, zo, wc) in groups:
                            for kk_ in range(8):
                                S.op("pe", lambda e, zt=zt, zo=zo, wc=wc, kk_=kk_: e.matmul(zt[:, zo:zo + 256], lhsT=hT[:, kk_, cs], rhs=win[:, kk_, wc:wc + 256], start=(kk_ == 0), stop=(kk_ == 7)),
                                     r=["winD"], w=["zA" if zt is zA else "zB"])
                        S.op("act", lambda e: e.activation(out=w["f"][:], in_=zA[:, 256:512], func=AF.Exp, scale=-1.0), r=["zA"], w=[kf])
                        S.op("act", lambda e: e.activation(out=w["q32"][:], in_=zA[:, 0:256], func=AF.Copy), r=["zA"], w=[("q32", d, b)])
                        S.op("act", lambda e: e.activation(out=vv[:], in_=zB[:, 0:256], func=AF.Copy), r=["zB"], w=[kv])
                        if d == 0 and need_out:
                            S.op("act", lambda e: e.activation(out=w["gtmp"][:], in_=zB[:, 256:512], func=AF.Exp, scale=-1.0), r=["zB"], w=[("gtmp", d, b)])
                            S.op("act", lambda e: e.activation(out=w["gtmp"][:], in_=w["gtmp"][:], func=AF.Ln, bias=c1[:, 0:1]), r=[("gtmp", d, b), "c1"], w=[("gtmp", d, b)])
                            S.op("act", lambda e: e.activation(out=w["gtmp"][:], in_=w["gtmp"][:], func=AF.Exp, scale=-1.0), r=[("gtmp", d, b)], w=[("gtmp", d, b)])
                            S.op("dve", lambda e: e.tensor_tensor(out=sgall[:, t, :], in0=zB[:, 256:512], in1=w["gtmp"][:], op=ALU.mult), r=["zB", ("gtmp", d, b)], w=["sgall"])
                        S.seg()
                        S.op("act", lambda e: e.activation(out=w["f"][:], in_=w["f"][:], func=AF.Ln, bias=c1[:, 0:1]), r=[kf, "c1"], w=[kf])
                        S.op("act", lambda e: e.activation(out=w["f"][:], in_=w["f"][:], func=AF.Exp, scale=-1.0), r=[kf], w=[kf])
                        if l > 0:
                            S.op("dve", lambda e: e.tensor_tensor(out=w["f"][:], in0=w["f"][:], in1=lbb[:, 1, d, :], op=ALU.mult), r=[kf, "lbb"], w=[kf])
                            S.op("dve", lambda e: e.tensor_tensor(out=w["f"][:], in0=w["f"][:], in1=lbb[:, 0, d, :], op=ALU.add), r=[kf, "lbb"], w=[kf])
                        S.op("act", lambda e: e.activation(out=w["lf"][:], in_=w["f"][:], func=AF.Ln), r=[kf], w=[klf])
                        S.op("pool", lambda e: e.tensor_scalar(out=w["kk"][:], in0=w["f"][:], scalar1=-1.0, scalar2=1.0, op0=ALU.mult, op1=ALU.add), r=[kf], w=[kkk])
                        S.seg()
                        for j in range(2):
                            S.op("pe", lambda e, j=j: e.matmul(bc[:, j, :], lhsT=tri[:, 3 * d + j, :], rhs=w["lf"][:], start=True, stop=True), r=[klf, "tri"], w=["bc"])
                        for c in range(2):
                            S.op("pe", lambda e, c=c: e.matmul(decp[:, c, :], lhsT=w["lf"][:, c * 128:(c + 1) * 128], rhs=CI[:, d, :], start=True, stop=True), r=[klf, "CI"], w=["decp"])
                        S.op("pe", lambda e: e.matmul(zB[:, 0:256], lhsT=tri[:, 3 * d + 2, :], rhs=w["lf"][:], start=True, stop=True), r=[klf, "tri"], w=["zB"])
                        S.op("act", lambda e: e.activation(out=w["dec"][:], in_=decp, func=AF.Exp), r=["decp"], w=[("dec", d, b)])
                        S.op("act", lambda e: e.activation(out=w["ex"][:, 0, :], in_=bc[:, 0, :], func=AF.Exp), r=["bc", ("ex", d, 0)], w=[("ex", d, 0)])
                        S.op("act", lambda e: e.activation(out=w["ex"][:, 1, :], in_=bc[:, 0, :], func=AF.Exp, scale=-1.0), r=["bc"], w=[("ex", d, 1)])
                        S.op("act", lambda e: e.activation(out=w["ex"][:, 2, :], in_=bc[:, 1, :], func=AF.Exp), r=["bc"], w=[("ex", d, 2)])
                        S.op("dve", lambda e: e.scalar_tensor_tensor(out=w["qt"][:, 0, :], in0=w["q32"][:], scalar=tokm[:, d, 2:3], in1=w["ex"][:, 0, :], op0=ALU.mult, op1=ALU.mult), r=[("q32", d, b), ("ex", d, 0), "tokm"], w=[("qt", d, b)])
                        S.op("dve", lambda e: e.scalar_tensor_tensor(out=w["qt"][:, 2, :], in0=w["kk"][:], scalar=tokm[:, d, 0:1], in1=w["ex"][:, 1, :], op0=ALU.mult, op1=ALU.mult), r=[kkk, ("ex", d, 1), "tokm"], w=[("qt", d, b)])
                        S.op("dve", lambda e: e.scalar_tensor_tensor(out=w["qt"][:, 1, :], in0=w["q32"][:], scalar=tokm[:, d, 3:4], in1=w["ex"][:, 2, :], op0=ALU.mult, op1=ALU.mult), r=[("q32", d, b), ("ex", d, 2), "tokm"], w=[("qt", d, b)])
                        S.op("act", lambda e: e.activation(out=w["ex"][:, 0, :], in_=bc[:, 1, :], func=AF.Exp, scale=-1.0), r=["bc"], w=[("ex", d, 0)])
                        S.op("act", lambda e: e.activation(out=w["ex"][:, 1, :], in_=zB[:, 0:256], func=AF.Exp), r=["zB"], w=[("ex", d, 1)])
                        S.op("pool", lambda e: e.tensor_tensor(out=w["qt"][:, 3, :], in0=w["kk"][:], in1=w["ex"][:, 0, :], op=ALU.mult), r=[kkk, ("ex", d, 0)], w=[("qt", d, b)])
                        for a_ in range(2):
                            S.op("dve", lambda e, a_=a_: e.scalar_tensor_tensor(out=w["kh"][:, a_, :], in0=w["kk"][:], scalar=tokm[:, d, 4 + a_:5 + a_], in1=w["ex"][:, 1, :], op0=ALU.mult, op1=ALU.mult), r=[kkk, ("ex", d, 1), "tokm"], w=[("kh", d, b)])
                        S.seg()
                        for j in range(8):
                            S.op("pe", lambda e, j=j: e.transpose(out=tp[:, j, :], in_=w["qt"][:, j // 2, (j % 2) * 128:(j % 2 + 1) * 128], identity=identb[:]), r=[("qt", d, b), "identb"], w=["tp"])
                        S.op("act", lambda e: e.activation(out=w["qkT"][:], in_=tp[:, 0:4, :], func=AF.Copy), r=["tp"], w=[("qkT", d, b)])
                        S.op("act", lambda e: e.activation(out=w["kTp"][0:64, 0, :, :], in_=tp[0:64, 4:8, :], func=AF.Copy), r=["tp"], w=[("kTp", d, b)])
                        S.op("dve", lambda e: e.tensor_copy(out=w["kTp"][64:128, 1, :, :], in_=tp[64:128, 4:8, :]), r=["tp"], w=[("kTp", d, b)])

                    def chain(t, d, b, need_out, it):
                        w = W[d, b]
                        vv = w["v"][it % 2]
                        kv = ("v", d, it % 2)
                        o_ = op2[d]
                        if need_out:
                            for h in range(4):
                                c, hb = h // 2, 64 * (h % 2)
                                hh = h % 2
                                S.op("pe", lambda e, h=h, c=c, hh=hh: e.matmul(attp[:, h, :], lhsT=w["kTp"][:, hh, c, :], rhs=w["qkT"][:, c, :], start=True, stop=False),
                                     r=[("qkT", d, b), ("kTp", d, b)], w=["attp"])
                                S.op("pe", lambda e, h=h, c=c, hh=hh: e.matmul(attp[:, h, :], lhsT=w["kTp"][:, hh, 2 + c, :], rhs=w["qkT"][:, 2 + c, :], start=False, stop=True),
                                     r=[("qkT", d, b), ("kTp", d, b)], w=["attp"])
                            S.op("dve", lambda e: e.copy_predicated(out=w["attm"][:], mask=vm8[:, d], data=attp[:]), r=["attp", "vm8"], w=[("attm", d, b)])
                            S.seg()
                            for h in range(4):
                                S.op("pe", lambda e, h=h: e.matmul(o_[:, h * 64:(h + 1) * 64], lhsT=w["attm"][:, h, :], rhs=vv[:, h * 64:(h + 1) * 64], start=(h == 0), stop=False, skip_group_check=True),
                                     r=[("attm", d, b), kv], w=[("oD", d)])
                        for a in ((0, 1) if d == 0 else (1, 0)):
                            cur = scur[d]
                            Sc = S32[d][cur]
                            Sn = S32[d][1 - cur]
                            scur[d] = 1 - cur
                            if need_out:
                                for hh in range(2):
                                    hb = 64 * hh
                                    S.op("pool", lambda e, hh=hh, hb=hb, Sc=Sc: e.tensor_copy(out=Sbf[d][hb:hb + 64, 0, :, hh, :], in_=Sc[hb:hb + 64, :, :]), r=[("S32", d, cur)], w=[("Sbf", d, 0, hh)])
                                    S.op("dve", lambda e, hh=hh, hb=hb, a=a, Sc=Sc: e.tensor_tensor(out=Sbf[d][hb:hb + 64, 1, :, hh, :], in0=Sc[hb:hb + 64, :, :], in1=w["dec"][hb:hb + 64, :, 2 + a:3 + a].to_broadcast([64, 2, 64]), op=ALU.mult), r=[("S32", d, cur), ("dec", d, b)], w=[("Sbf", d, 1, hh)])
                                S.seg()
                                for h in range(4):
                                    c, hh = h // 2, h % 2
                                    for xy in range(2):
                                        S.op("pe", lambda e, h=h, c=c, hh=hh, a=a, xy=xy: e.matmul(o_[a * 64:(a + 1) * 64, h * 64:(h + 1) * 64], lhsT=w["qkT"][:, xy * 2 + c, a * 64:(a + 1) * 64], rhs=Sbf[d][:, xy, c, hh, :], start=False, stop=True, skip_group_check=True),
                                             r=[("qkT", d, b), ("Sbf", d, xy, hh)], w=[("oD", d)])
                            for c in range(2):
                                for hh in range(2):
                                    h = 2 * c + hh
                                    S.op("pe", lambda e, h=h, c=c, hh=hh, a=a: e.matmul(Pps[:, c, hh, :], lhsT=w["kh"][:, a, c * 128:(c + 1) * 128], rhs=vv[:, h * 64:(h + 1) * 64], start=True, stop=True, skip_group_check=True),
                                         r=[("kh", d, b), kv], w=["Pps"])
                            for c in range(2):
                                for hh in range(2):
                                    hb = 64 * hh
                                    S.op("dve", lambda e, c=c, hh=hh, hb=hb, a=a, Sc=Sc, Sn=Sn: e.scalar_tensor_tensor(out=Sn[hb:hb + 64, c, :], in0=Sc[hb:hb + 64, c, :], scalar=w["dec"][hb:hb + 64, c, a:a + 1], in1=Pps[hb:hb + 64, c, hh, :], op0=ALU.mult, op1=ALU.add),
                                         r=[("S32", d, cur), ("dec", d, b), "Pps"], w=[("S32", d, 1 - cur)])
                            S.seg()
                        if need_out:
                            S.op("dve", lambda e: e.tensor_tensor(out=osum[:, t, :], in0=osum[:, t, :], in1=o_[:, :], op=ALU.add), r=[("oD", d), ("osum", t)], w=[("osum", t)])

                    order_f = [16, 17] + list(range(16))
                    order_b = [17, 16] + list(range(15, -1, -1))
                    def rec_prep(i):
                        return [S.split(S.record(lambda d=d, t=t: prep(t, d, 0, ctx_out or t < 16, i))) for d, t in ((0, order_f[i]), (1, order_b[i]))]

                    S.emit_segs(rec_prep(0))
                    for i in range(NT):
                        pair = ((0, order_f[i]), (1, order_b[i]))
                        nxt = rec_prep(i + 1) if i + 1 < NT else None
                        if nxt is not None:
                            S.emit_segs([p[:1] for p in nxt])
                        S.emit_interleaved([S.record(lambda d=d, t=t: chain(t, d, 0, ctx_out or t < 16, i)) for d, t in pair])
                        if nxt is not None:
                            S.emit_segs([p[1:] for p in nxt])
                    S.barrier()
                    with contextlib.ExitStack() as ph2:
                        sq = [W[0, 0]["ex"][:, i, :] for i in range(2)]
                        ssD = SBT(ph2, "ssD", [128, 2, 8])
                        yv = [W[0, 0]["qt"][:, i, :] for i in range(2)]
                        for t in range(nqt):
                            t2 = t % 2
                            o3 = osum[:, t, :].rearrange("p (h e) -> p h e", e=64)
                            S.op("pool", lambda e, t2=t2, t=t: e.tensor_tensor(out=sq[t2], in0=osum[:, t, :], in1=osum[:, t, :], op=ALU.mult), r=[], w=[("sqD", t2)])
                            S.op("dve", lambda e, t2=t2: e.tensor_reduce(out=ssD[:, t2, 0:4], in_=sq[t2].rearrange("p (h e) -> p h e", e=64), axis=AX.X, op=ALU.add), r=[("sqD", t2)], w=[("ssD", t2)])
                            S.op("act", lambda e, t2=t2: e.activation(out=ssD[:, t2, 4:8], in_=ssD[:, t2, 0:4], func=AF.Sqrt, scale=1.0 / 64, bias=epsb[:, 0:1]), r=[("ssD", t2), "epsb"], w=[("ssD", t2)])
                            S.op("dve", lambda e, t2=t2: e.reciprocal(out=ssD[:, t2, 4:8], in_=ssD[:, t2, 4:8]), r=[("ssD", t2)], w=[("ssD", t2)])
                            S.op("dve", lambda e, t2=t2, o3=o3: e.tensor_tensor(out=sq[t2].rearrange("p (h e) -> p h e", e=64), in0=o3, in1=ssD[:, t2, 4:8].unsqueeze(2).to_broadcast([128, 4, 64]), op=ALU.mult), r=[("ssD", t2), ("sqD", t2)], w=[("sqD", t2)])
                            S.op("pool", lambda e, t2=t2: e.tensor_tensor(out=sq[t2], in0=sq[t2], in1=gnb[:], op=ALU.mult), r=[("sqD", t2), "gnb"], w=[("sqD", t2)])
                            S.op("pool", lambda e, t2=t2, t=t: e.tensor_tensor(out=yv[t2], in0=sq[t2], in1=sgall[:, t, :], op=ALU.mult), r=[("sqD", t2)], w=[("yvD", t2)])
                            for j in range(2):
                                S.op("pe", lambda e, t2=t2, j=j: e.transpose(out=tp[:, j, :], in_=yv[t2][:, j * 128:(j + 1) * 128], identity=identb[:]), r=[("yvD", t2), "identb"], w=["tp"])
                            S.op("act", lambda e, t=t: e.activation(out=yTm[:, :, t * 128:(t + 1) * 128], in_=tp[:, 0:2, :], func=AF.Copy), r=["tp"], w=["yTm"])
                S.barrier()
                dump("yTmD%d" % l, yTm[:])
                with contextlib.ExitStack() as ph5:
                    proj_out(ph5, l, 3, yTm, nblk)
            S.barrier()

        def dump(name, ap_sb, shape_note=None):
            if name in dbg_d:
                S.barrier()
                S.dma("sp", dbg_d[name], ap_sb, w=["dbgout"])
                S.barrier()


        for l in range(layers):
            ctx_out = l < DEPTH - 1
            modT = modTs[l]
            gsc = gscs[l]
            dump("modT%d" % l, modT[:])
            rmsnorm_to_hT(0, 5, router=False)
            dump("hT%d" % l, None)
            if stop_after == ("norm1", l):
                break
            if "A" in mixers:
                mixer_lru(l, ctx_out)
            if "B" in mixers:
                mixer_attn(l, ctx_out)
            if "C" in mixers:
                mixer_sg(l, ctx_out)
            if "D" in mixers:
                mixer_hgrn(l, ctx_out)
            if skip_moe:
                continue
            with contextlib.ExitStack() as phg:
                gT = SBT(phg, "gT", [NE, TT])
                rmsnorm_to_hT(1, 5 if ctx_out else 4, router=True, gT=gT)
                dump("gT%d" % l, gT[:])
                moe(l, 5 if ctx_out else 4, gT)
            dump("xT%d" % l, xT[:])

        with contextlib.ExitStack() as ph:
            sq = [SBT(ph, "fsq%d" % i, [128, 8, 512], BF16) for i in range(2)]
            rs = [SBT(ph, "frs%d" % i, [128, 512], F32) for i in range(2)]
            yb = [SBT(ph, "fyb%d" % i, [128, 8, 512], F32) for i in range(2)]
            ost = [SBT(ph, "ost%d" % i, [128, D], F32) for i in range(2)]
            ssp = [PST(ph, "fssp%d" % i, [128, 512], F32) for i in range(2)]
            tp = [PST(ph, "ftp%d" % i, [128, 4, 128], F32) for i in range(2)]
            n_tp = 0
            for bi in range(4):
                t0, n = blocks[bi]
                b2 = bi % 2
                S.op("act", lambda e, b2=b2, t0=t0, n=n: e.activation(out=sq[b2][:, :, 0:n], in_=xT[:, :, t0:t0 + n], func=AF.Square), r=[("xT", bi)], w=[("sq", b2)])
                for c in range(8):
                    S.op("pe", lambda e, b2=b2, c=c, n=n: e.matmul(ssp[b2][:, 0:n], lhsT=ones_b[:], rhs=sq[b2][:, c, 0:n], start=(c == 0), stop=(c == 7)),
                         r=[("sq", b2), "ones_b"], w=[("ssp", b2)])
                S.op("act", lambda e, b2=b2, n=n: e.activation(out=rs[b2][:, 0:n], in_=ssp[b2][:, 0:n], func=AF.Ln, scale=1.0 / D, bias=epsb[:, 0:1]),
                     r=[("ssp", b2), "epsb"], w=[("rs", b2)])
                S.op("act", lambda e, b2=b2, n=n: e.activation(out=rs[b2][:, 0:n], in_=rs[b2][:, 0:n], func=AF.Exp, scale=-0.5), r=[("rs", b2)], w=[("rs", b2)])
                for c in range(8):
                    S.op("dve", lambda e, c=c, t0=t0, n=n, b2=b2: e.scalar_tensor_tensor(out=yb[b2][:, c, 0:n], in0=xT[:, c, t0:t0 + n], scalar=gvec[:, 2, c:c + 1], in1=rs[b2][:, 0:n], op0=ALU.mult, op1=ALU.mult),
                         r=[("xT", bi), "gvec2", ("rs", b2)], w=[("yb", b2, c)])
                for ti in range(4):
                    tk = t0 + ti * 128
                    o2 = (tk // 128) % 2
                    for half in range(2):
                        p = tp[n_tp % 2]
                        for j in range(4):
                            c = half * 4 + j
                            S.op("pe", lambda e, p=p, j=j, c=c, b2=b2, ti=ti: e.transpose(out=p[:, j, :], in_=yb[b2][:, c, ti * 128:(ti + 1) * 128], identity=ident[:]),
                                 r=[("yb", b2, c), "ident"], w=[("ftp", n_tp % 2)])
                        if n_tp % 2 == 0:
                            S.op("act", lambda e, p=p, o2=o2, half=half: e.activation(out=ost[o2][:, half * 512:(half + 1) * 512], in_=p[:].rearrange("p a b -> p (a b)"), func=AF.Copy),
                                 r=[("ftp", n_tp % 2)], w=[("ost", o2)])
                        else:
                            S.op("dve", lambda e, p=p, o2=o2, half=half: e.tensor_copy(out=ost[o2][:, half * 512:(half + 1) * 512], in_=p[:].rearrange("p a b -> p (a b)")),
                                 r=[("ftp", n_tp % 2)], w=[("ost", o2)])
                        n_tp += 1
                    S.dma("sp", out_d[tk:tk + 128, :], ost[o2][:], r=[("ost", o2)])
            S.finish("sp")
    k.n_inst = S.n_inst
    return nc


_PERM = _perm_rope()


def _prep_shared(inp):
    f = lambda a: np.ascontiguousarray(np.asarray(a, dtype=np.float32))
    sh = {}
    sh["w_ada"] = f(inp["w_ada"])
    sh["b_adaT"] = f(np.asarray(inp["b_ada"]).reshape(DEPTH, 48, 128).transpose(0, 2, 1))
    sh["n1g"] = f(np.asarray(inp["norm1_g"]).reshape(DEPTH, 8, 128).transpose(0, 2, 1))
    sh["n2g"] = f(np.asarray(inp["norm2_g"]).reshape(DEPTH, 8, 128).transpose(0, 2, 1))
    sh["fng"] = f(np.asarray(inp["final_norm_g"]).reshape(8, 128).T)
    w_in = np.asarray(inp["w_in"])
    sh["w_in"] = f(w_in)
    qk = w_in[:, :, 512:1024]
    sh["w_inr"] = f(np.concatenate([qk[:, :, 0:256][:, :, _PERM], qk[:, :, 256:512][:, :, _PERM]], axis=2))
    sh["w_out"] = f(inp["w_out"])
    sh["router_w"] = f(np.asarray(inp["router_w"]).reshape(8, 128, NE).transpose(1, 0, 2))
    sh["router_b"] = f(np.asarray(inp["router_b"]).reshape(1, NE))
    sh["moe_w1"] = f(inp["moe_w1"])
    sh["moe_w3"] = f(inp["moe_w3"])
    sh["moe_w2"] = f(inp["moe_w2"])
    sh["lru_cw"] = f(np.asarray(inp["lru_conv_w"]).reshape(DEPTH, 4, 2, 128).transpose(0, 3, 2, 1))
    sh["lru_cb"] = f(np.asarray(inp["lru_conv_b"]).reshape(DEPTH, 2, 128).transpose(0, 2, 1))
    gbs = np.stack([np.asarray(inp["lru_br"]), np.asarray(inp["lru_bi"]), np.asarray(inp["lru_lam"])], axis=1)
    sh["lru_gb"] = f(gbs.reshape(DEPTH, 3, 2, 2, 128).transpose(0, 4, 1, 2, 3))
    wbd = np.zeros((DEPTH, 128, 2, 2, 2, 128), np.float32)
    for gi, nm in enumerate(("lru_wr", "lru_wi")):
        w = np.asarray(inp[nm])
        for d in range(2):
            for hh in range(4):
                cc, h2 = hh // 2, hh % 2
                wbd[:, h2 * 64:(h2 + 1) * 64, gi, d, cc, h2 * 64:(h2 + 1) * 64] = w[:, d, hh]
    sh["lru_wbd"] = wbd
    sh["sg_wT"] = f(np.asarray(inp["sg_w"]).transpose(0, 3, 1, 2))
    sh["sg_bT"] = f(np.asarray(inp["sg_b"]).transpose(0, 2, 1))
    sh["sg_g"] = f(inp["sg_norm_g"])
    sh["da_lam"] = f(np.asarray(inp["da_lam"]).reshape(DEPTH, 128))
    sh["da_g"] = f(inp["da_subln_g"])
    sh["m4"] = f((np.arange(128)[:, None] // 32) == np.arange(4)[None, :])
    cosT, sinT = _rope_tables()
    sh["cosT"] = f(cosT)
    sh["sinT"] = f(sinT)
    sI = np.arange(128)[:, None]
    tI = np.arange(128)[None, :]
    same = (sI // 64) == (tI // 64)
    sl = sI % 64
    tl = tI % 64
    F_ = lambda m: m.astype(np.float32)
    sh["tri"] = f(np.stack([
        F_(same & (tl <= 31) & (sI <= tI)), F_(same & (sI <= tI)) - F_(same & (sl <= 31)), F_(same & (sI > tI)),
        F_(same & (tl >= 32) & (sI >= tI)), F_(same & (sI >= tI)) - F_(same & (sl >= 32)), F_(same & (sI < tI))], axis=1))
    pl_ = np.arange(128) % 64
    mPf, mPb = F_(pl_ <= 31), F_(pl_ >= 32)
    chA, chB = F_(np.arange(128) < 64), F_(np.arange(128) >= 64)
    sh["tokm"] = f(np.stack([np.stack([mPf, 1 - mPf, 0.125 * mPf, 0.125 * (1 - mPf), chA, chB], axis=1), np.stack([mPb, 1 - mPb, 0.125 * mPb, 0.125 * (1 - mPb), chA, chB], axis=1)], axis=1))
    sh["vmask"] = f(np.stack([same & (sI <= tI), same & (sI >= tI)], axis=1))
    pch = (np.arange(128)[:, None] // 64) == np.arange(2)[None, :]
    pl = (np.arange(128) % 64)[:, None]
    sh["ci"] = f(np.stack([np.concatenate([pch, pch & (pl <= 31)], axis=1), np.concatenate([pch, pch & (pl >= 32)], axis=1)], axis=1))
    sh["hg_g"] = f(inp["hg_norm_g"])
    sh["hg_lb"] = f(np.asarray(inp["hg_lb"]).reshape(1, -1))
    sh["ident"] = np.eye(128, dtype=np.float32)
    selm = np.zeros((NE, NE, 128), np.float32)
    for e in range(NE):
        selm[e, e, :] = 1.0
    sh["sel"] = selm
    return sh


def _prep_core(inp, b):
    f = lambda a: np.ascontiguousarray(np.asarray(a, dtype=np.float32))
    m = {}
    m["xin"] = f(np.concatenate([np.asarray(inp["x"])[b], np.asarray(inp["ctx"])[b]], axis=0))
    cc = np.stack([np.asarray(inp["c"])[b], np.asarray(inp["c_ctx"])], axis=1)
    m["cT"] = f(cc.reshape(8, 128, 2).transpose(1, 0, 2))
    return m


_NC_CACHE = {}


def kernel(**inputs):
    nc = build_nc()
    sh = _prep_shared(inputs)
    in_maps = []
    for b in range(8):
        m = dict(sh)
        m.update(_prep_core(inputs, b))
        in_maps.append(m)
    res = run_bass_kernel_spmd(nc, in_maps, core_ids=list(range(8)))
    out = np.stack([np.asarray(r["out"], dtype=np.float32) for r in res.results], axis=0)
    return out
```

```python
import contextlib
import math
import numpy as np
import concourse.bass as bass
import concourse.mybir as mybir
from concourse.bass_utils import run_bass_kernel_spmd

F32 = mybir.dt.float32
BF16 = mybir.dt.bfloat16
AF = mybir.ActivationFunctionType
ALU = mybir.AluOpType
AX = mybir.AxisListType

D = 1024
T = 2048
TC = 256
TT = T + TC
NT = TT // 128
DEPTH = 2
EPS = 1e-6
NE = 16
DE = 512
import os as _os
KCUT = int(_os.environ.get('KCUT', '0'))


class _Eng:
    def __init__(self, S, name, h):
        self.S = S
        self.name = name
        self.h = h
        self.sem = S.es.enter_context(S.nc.semaphore("sem_" + name))
        self.count = 0
        self.seen = {}


class _DSem:
    def __init__(self, S, name):
        self.name = name
        self.sem = S.es.enter_context(S.nc.semaphore(name))
        self.count = 0


class Sched:
    def __init__(self, nc, es):
        self.nc = nc
        self.es = es
        self.E = {
            "pe": _Eng(self, "pe", nc.tensor),
            "act": _Eng(self, "act", nc.scalar),
            "dve": _Eng(self, "dve", nc.vector),
            "pool": _Eng(self, "pool", nc.gpsimd),
            "sp": _Eng(self, "sp", nc.sync),
        }
        self.last_w = {}
        self.reads = {}
        self.dsems = {q: [_DSem(self, "dsem_%s_%d" % (q, i)) for i in range(12)] for q in ("sp", "act", "pool")}
        self.dnext = {q: 0 for q in self.dsems}
        self.n_inst = 0
        self.nrot = 0
        self.rec = None

    def _need(self, eng, deps):
        for obj, c in deps.items():
            if c <= 0:
                continue
            if obj is eng and eng.name in ("pe", "sp"):
                continue
            if eng.seen.get(obj, 0) >= c:
                continue
            eng.h.wait_ge(obj.sem, c)
            eng.seen[obj] = c

    def _deps(self, r, w):
        deps = {}
        for k in r:
            lw = self.last_w.get(k)
            if lw is not None:
                deps[lw[0]] = max(deps.get(lw[0], 0), lw[1])
        for k in w:
            lw = self.last_w.get(k)
            if lw is not None:
                deps[lw[0]] = max(deps.get(lw[0], 0), lw[1])
            for o, c in self.reads.get(k, {}).items():
                deps[o] = max(deps.get(o, 0), c)
        return deps

    def _mark(self, obj, cnt, r, w):
        for k in r:
            self.reads.setdefault(k, {})[obj] = cnt
        for k in w:
            self.last_w[k] = (obj, cnt)
            self.reads[k] = {}

    def op(self, en, fn, r=(), w=()):
        if self.rec is not None:
            self.rec.append((en, fn, tuple(r), tuple(w)))
            return None
        eng = self.E[en]
        self._need(eng, self._deps(r, w))
        inst = fn(eng.h)
        eng.count += 1
        inst.then_inc(eng.sem, 1)
        self._mark(eng, eng.count, r, w)
        self.n_inst += 1
        return inst

    def dma(self, q, out, in_, r=(), w=()):
        eng = self.E[q]
        deps = self._deps(r, w)
        lst = self.dsems[q]
        ds = lst[self.dnext[q] % len(lst)]
        self.dnext[q] += 1
        if ds.count:
            deps[ds] = max(deps.get(ds, 0), ds.count)
        self._need(eng, deps)
        inst = eng.h.dma_start(out=out, in_=in_)
        ds.count += 16
        inst.then_inc(ds.sem, 16)
        self._mark(ds, ds.count, r, w)
        self.n_inst += 1
        return inst

    def record(self, f):
        assert self.rec is None
        self.rec = []
        f()
        lst, self.rec = self.rec, None
        return lst

    def seg(self):
        if self.rec is not None:
            self.rec.append(None)

    @staticmethod
    def split(l):
        cur, out = [], []
        for it in l:
            if it is None:
                if cur:
                    out.append(cur)
                cur = []
            else:
                cur.append(it)
        if cur:
            out.append(cur)
        return out

    def emit_interleaved(self, lists):
        self.emit_segs([self.split(l) for l in lists])

    def emit_segs(self, segs):
        k = 0
        while any(k < len(s) for s in segs):
            for s in segs:
                if k < len(s):
                    for en, fn, r, w in s[k]:
                        self.op(en, fn, r, w)
            k += 1

    def barrier(self):
        objs = list(self.E.values())
        dl = [d for lst in self.dsems.values() for d in lst if d.count]
        for e in objs:
            deps = {o: o.count for o in objs if o is not e}
            for d in dl:
                deps[d] = d.count
            self._need(e, deps)
        self.last_w = {}
        self.reads = {}
        for e in objs:
            if e.count > 3000:
                self.nrot += 1
                e.sem = self.es.enter_context(self.nc.semaphore("sem_%s_r%d" % (e.name, self.nrot)))
                e.count = 0
                for o in objs:
                    o.seen.pop(e, None)

    def finish(self, eng_name="sp"):
        e = self.E[eng_name]
        deps = {o: o.count for o in self.E.values() if o is not e}
        for lst in self.dsems.values():
            for d in lst:
                if d.count:
                    deps[d] = d.count
        self._need(e, deps)


def _perm_rope():
    p = np.arange(256)
    half = (p // 8) % 2
    return np.where(half == 0, p + 8, p - 8)


def _rope_tables():
    rows = T // 64
    row_ids = np.repeat(np.arange(rows, dtype=np.float32), 64)
    col_ids = np.tile(np.arange(64, dtype=np.float32), rows)
    freqs = (np.float32(10000.0) ** (-np.arange(8, dtype=np.float32) / np.float32(8))).astype(np.float32)
    ang = np.stack([row_ids[:, None] * freqs, col_ids[:, None] * freqs], axis=1).astype(np.float32)
    cos = np.cos(ang).astype(np.float32)
    sin = np.sin(ang).astype(np.float32)
    p = np.arange(128)
    axis = (p // 16) % 2
    half = (p // 8) % 2
    f = p % 8
    cosT = cos[:, axis, f].T.copy()
    sgn = np.where(half == 0, -1.0, 1.0).astype(np.float32)
    sinT = (sin[:, axis, f].T * sgn[:, None]).astype(np.float32).copy()
    return cosT, sinT


class K:
    pass


def build_nc(stop_after=None, dbg=(), mixers="ABCD", skip_moe=bool(int(_os.environ.get('KSKIPMOE', '0'))), layers=DEPTH):
    nc = bass.Bass("TRN2", target_bir_lowering=False)
    es = contextlib.ExitStack()
    k = K()
    k.nc = nc
    dram = {}

    def din(name, shape, dt=F32):
        dram[name] = nc.dram_tensor(name, list(shape), dt, kind="ExternalInput").ap()
        return dram[name]

    def dout(name, shape, dt=F32):
        if name.startswith("dbg_yTm"):
            dt = BF16
        dram[name] = nc.dram_tensor(name, list(shape), dt, kind="ExternalOutput").ap()
        return dram[name]

    xin = din("xin", [TT, D])
    cT = din("cT", [128, 8, 2])
    w_ada = din("w_ada", [DEPTH, D, 6 * D])
    b_adaT = din("b_adaT", [DEPTH, 128, 48])
    n1g = din("n1g", [DEPTH, 128, 8])
    n2g = din("n2g", [DEPTH, 128, 8])
    fng = din("fng", [128, 8])
    w_in = din("w_in", [DEPTH, D, 3072])
    w_inr = din("w_inr", [DEPTH, D, 512])
    w_out = din("w_out", [DEPTH, D, D])
    router_w = din("router_w", [128, 8, NE])
    router_b = din("router_b", [1, NE])
    moe_w1 = din("moe_w1", [DEPTH, NE, D, DE])
    moe_w3 = din("moe_w3", [DEPTH, NE, D, DE])
    moe_w2 = din("moe_w2", [DEPTH, NE, DE, D])
    lru_cw = din("lru_cw", [DEPTH, 128, 2, 4])
    lru_cb = din("lru_cb", [DEPTH, 128, 2])
    lru_gb = din("lru_gb", [DEPTH, 128, 3, 2, 2])
    lru_wbd = din("lru_wbd", [DEPTH, 128, 2, 2, 2, 128])
    sg_wT = din("sg_wT", [DEPTH, 128, 4, 128])
    sg_bT = din("sg_bT", [DEPTH, 128, 4])
    sg_g = din("sg_g", [DEPTH, 256])
    da_lam = din("da_lam", [DEPTH, 128])
    da_g = din("da_g", [DEPTH, 64])
    m4_d = din("m4", [128, 4])
    cosT_d = din("cosT", [128, T])
    sinT_d = din("sinT", [128, T])
    tri_d = din("tri", [128, 6, 128])
    tokm_d = din("tokm", [128, 2, 6])
    ci_d = din("ci", [128, 2, 4])
    vmask_d = din("vmask", [128, 2, 128])
    hg_g = din("hg_g", [DEPTH, 256])
    hg_lb_d = din("hg_lb", [1, 2 * DEPTH * 256])
    ident_d = din("ident", [128, 128])
    sel_d = din("sel", [NE, NE, 128])
    out_d = dout("out", [T, D])
    dbg_d = {}
    for nm, shp in dbg:
        dbg_d[nm] = dout("dbg_" + nm, shp)

    with es:
        S = Sched(nc, es)

        uid = [0]

        def SBT(ph, name, shape, dt=F32):
            uid[0] += 1
            return ph.enter_context(nc.sbuf_tensor("sb%d_%s" % (uid[0], name), list(shape), dt))

        def PST(ph, name, shape, dt=F32):
            uid[0] += 1
            return ph.enter_context(nc.psum_tensor("ps%d_%s" % (uid[0], name), list(shape), dt))

        def sb(name, shape, dt=F32):
            return SBT(es, name, shape, dt)

        xT = sb("xT", [128, 8, TT])
        hT = sb("hT", [128, 8, TT], BF16)
        ident = sb("ident", [128, 128])
        identb = sb("identb", [128, 128], BF16)
        ones_b = sb("ones_b", [128, 128], BF16)
        scT = sb("scT", [128, 8, 2])
        modTs = [sb("modT%d" % i, [128, 48, 2]) for i in range(DEPTH)]
        gvec = sb("gvec", [128, 3, 8])
        gscs = [sb("gsc%d" % i, [128, 2, 8, 2]) for i in range(DEPTH)]
        scTb = sb("scTb", [128, 8, 2], BF16)
        modT = modTs[0]
        gsc = gscs[0]
        rw = sb("rw", [128, 8, NE])
        rb = sb("rb", [128, NE])

        S.dma("sp", ident[:], ident_d[:, :], w=["ident"])
        S.dma("sp", scT[:], cT[:, :, :], w=["scT"])
        S.dma("sp", rw[:], router_w[:, :, :], w=["rw"])
        S.dma("sp", rb[:], router_b.partition_broadcast(128), w=["rb"])
        S.dma("sp", gvec[:, 2, :], fng[:, :], w=["gvec2"])
        S.op("dve", lambda e: e.tensor_copy(out=identb[:], in_=ident[:]), r=["ident"], w=["identb"])
        S.op("dve", lambda e: e.memset(ones_b[:], 1.0), w=["ones_b"])
        S.op("act", lambda e: e.activation(out=scT[:], in_=scT[:], func=AF.Silu), r=["scT"], w=["scT"])
        S.op("dve", lambda e: e.tensor_copy(out=scTb[:], in_=scT[:]), r=["scT"], w=["scTb"])

        with contextlib.ExitStack() as ph:
            stg = [SBT(ph, "stg%d" % i, [128, D], F32) for i in range(3)]
            tp = [PST(ph, "tp%d" % i, [128, 4, 128], F32) for i in range(2)]
            wa = [SBT(ph, "wa%d" % i, [128, 8, 512], BF16) for i in range(4)]
            mps = [PST(ph, "mp%d" % i, [128, 48, 2], F32) for i in range(DEPTH)]
            badaTs = [SBT(ph, "badaT%d" % i, [128, 48]) for i in range(DEPTH)]
            gvecs = [SBT(ph, "gvecs%d" % i, [128, 2, 8]) for i in range(DEPTH)]

            def gen_xload():
                n = 0
                for t in range(NT):
                    st = stg[t % 3]
                    S.dma("sp", st[:], xin[t * 128:(t + 1) * 128, :], w=[("stg", t % 3)])
                    for half in range(2):
                        p = tp[n % 2]
                        for j in range(4):
                            c = half * 4 + j
                            S.op("pe", lambda e, p=p, j=j, c=c, st=st: e.transpose(out=p[:, j, :], in_=st[:, c * 128:(c + 1) * 128], identity=ident[:]),
                                 r=[("stg", t % 3), "ident"], w=[("tp", n % 2)])
                        if n % 2 == 0:
                            S.op("act", lambda e, p=p, half=half, t=t: e.activation(out=xT[:, half * 4:half * 4 + 4, t * 128:(t + 1) * 128], in_=p[:], func=AF.Copy),
                                 r=[("tp", n % 2)], w=[("xT", t // 4)])
                        else:
                            S.op("dve", lambda e, p=p, half=half, t=t: e.tensor_copy(out=xT[:, half * 4:half * 4 + 4, t * 128:(t + 1) * 128], in_=p[:]),
                                 r=[("tp", n % 2)], w=[("xT", t // 4)])
                        n += 1
                    yield

            def gen_adaln():
                nw = 0
                for l in range(DEPTH):
                    mp = mps[l]
                    S.dma("act", badaTs[l][:], b_adaT[l, :, :], w=[("badaT", l)])
                    S.dma("act", gvecs[l][:, 0, :], n1g[l, :, :], w=[("gvecs", l)])
                    S.dma("act", gvecs[l][:, 1, :], n2g[l, :, :], w=[("gvecs", l)])
                    for jb in range(12):
                        w4 = nw % 4
                        nw += 1
                        wt = wa[w4]
                        S.dma("pool", wt[:], w_ada[l, :, jb * 512:(jb + 1) * 512].rearrange("(k p) n -> p k n", p=128), w=[("wa", w4)])
                        for jj in range(4):
                            j = jb * 4 + jj
                            for kk in range(8):
                                S.op("pe", lambda e, wt=wt, jj=jj, kk=kk, j=j, mp=mp: e.matmul(mp[:, j, :], lhsT=wt[:, kk, jj * 128:(jj + 1) * 128], rhs=scTb[:, kk, :], start=(kk == 0), stop=(kk == 7)),
                                     r=[("wa", w4), "scTb"], w=[("mp", l)])
                        yield
                    S.op("dve", lambda e, l=l, mp=mp: e.tensor_tensor(out=modTs[l][:], in0=mp[:], in1=badaTs[l][:].unsqueeze(2).to_broadcast([128, 48, 2]), op=ALU.add),
                         r=[("mp", l), ("badaT", l)], w=[("modT", l)])
                    for n_, off in ((0, 8), (1, 32)):
                        S.op("dve", lambda e, n_=n_, off=off, l=l: e.tensor_scalar(out=gscs[l][:, n_, :, :], in0=modTs[l][:, off:off + 8, :], scalar1=1.0, scalar2=None, op0=ALU.add),
                             r=[("modT", l)], w=[("gsc", l)])
                        S.op("dve", lambda e, n_=n_, l=l: e.tensor_tensor(out=gscs[l][:, n_, :, :], in0=gscs[l][:, n_, :, :], in1=gvecs[l][:, n_, :].unsqueeze(2).to_broadcast([128, 8, 2]), op=ALU.mult),
                             r=[("gsc", l), ("gvecs", l)], w=[("gsc", l)])
                    yield

            gens = [gen_xload(), gen_adaln()]
            while gens:
                for g in list(gens):
                    try:
                        next(g)
                    except StopIteration:
                        gens.remove(g)
        S.barrier()

        blocks = [(0, 512), (512, 512), (1024, 512), (1536, 512), (2048, 256)]

        def rmsnorm_to_hT(which, nblk, router, gT=None):
            shoff = 0 if which == 0 else 24
            with contextlib.ExitStack() as ph:
                sq = [SBT(ph, "sq%d" % i, [128, 8, 512], BF16) for i in range(2)]
                rs = [SBT(ph, "rs%d" % i, [128, 512], F32) for i in range(2)]
                tmp = [SBT(ph, "ntmp%d" % i, [128, 512], F32) for i in range(3)]
                ssp = [PST(ph, "ssp%d" % i, [128, 512], F32) for i in range(2)]
                if router:
                    hf = [SBT(ph, "hf%d" % i, [128, 8, 512], F32) for i in range(2)]
                    lgp = [PST(ph, "lgp%d" % i, [128, NE], F32) for i in range(2)]
                    gtp = [PST(ph, "gtp%d" % i, [NE, 128], F32) for i in range(2)]
                    rt = SBT(ph, "rt", [128, 2, 96], F32)
                    rsm = SBT(ph, "rsm", [128, 2, 8], F32)
                nt_ = 0
                pend = []
                for bi in range(nblk):
                    t0, n = blocks[bi]
                    s = 0 if t0 < T else 1
                    b2 = bi % 2
                    S.op("act", lambda e, b2=b2, t0=t0, n=n: e.activation(out=sq[b2][:, :, 0:n], in_=xT[:, :, t0:t0 + n], func=AF.Square),
                         r=[("xT", bi)], w=[("sq", b2)])
                    for c in range(8):
                        S.op("pe", lambda e, b2=b2, c=c, n=n: e.matmul(ssp[b2][:, 0:n], lhsT=ones_b[:], rhs=sq[b2][:, c, 0:n], start=(c == 0), stop=(c == 7)),
                             r=[("sq", b2), "ones_b"], w=[("ssp", b2)])
                    S.op("act", lambda e, b2=b2, n=n: e.activation(out=rs[b2][:, 0:n], in_=ssp[b2][:, 0:n], func=AF.Ln, scale=1.0 / D, bias=epsb[:, 0:1]),
                         r=[("ssp", b2), "epsb"], w=[("rs", b2)])
                    S.op("act", lambda e, b2=b2, n=n: e.activation(out=rs[b2][:, 0:n], in_=rs[b2][:, 0:n], func=AF.Exp, scale=-0.5), r=[("rs", b2)], w=[("rs", b2)])
                    for c in range(8):
                        tb = nt_ % 3
                        nt_ += 1
                        S.op("dve", lambda e, tb=tb, c=c, t0=t0, n=n, b2=b2, s=s: e.scalar_tensor_tensor(out=tmp[tb][:, 0:n], in0=xT[:, c, t0:t0 + n], scalar=gsc[:, which, c, s:s + 1], in1=rs[b2][:, 0:n], op0=ALU.mult, op1=ALU.mult),
                             r=[("xT", bi), "gsc", ("rs", b2)], w=[("ntmp", tb)])
                        if not router:
                            S.op("act", lambda e, tb=tb, c=c, t0=t0, n=n, s=s: e.activation(out=hT[:, c, t0:t0 + n], in_=tmp[tb][:, 0:n], func=AF.Identity, bias=modT[:, shoff + c, s:s + 1]),
                                 r=[("ntmp", tb), "modT"], w=[("hT", bi)])
                        else:
                            S.op("act", lambda e, tb=tb, c=c, n=n, s=s, b2=b2: e.activation(out=hf[b2][:, c, 0:n], in_=tmp[tb][:, 0:n], func=AF.Identity, bias=modT[:, shoff + c, s:s + 1]),
                                 r=[("ntmp", tb), "modT"], w=[("hf", b2, c)])
                            S.op("act", lambda e, tb=tb, c=c, t0=t0, n=n, s=s: e.activation(out=hT[:, c, t0:t0 + n], in_=tmp[tb][:, 0:n], func=AF.Identity, bias=modT[:, shoff + c, s:s + 1]),
                                 r=[("ntmp", tb), "modT"], w=[("hT", bi)])
                    if router:
                        for ti in range(n // 128):
                            tk = t0 + ti * 128
                            q2 = (tk // 128) % 2
                            for c in range(8):
                                S.op("pe", lambda e, c=c, ti=ti, b2=b2, q2=q2: e.matmul(lgp[q2][:, :], lhsT=hf[b2][:, c, ti * 128:(ti + 1) * 128], rhs=rw[:, c, :], start=(c == 0), stop=(c == 7)),
                                     r=[("hf", b2, c), "rw"], w=[("lgp", q2)])
                            R = rt[:, q2, :]
                            sm = rsm[:, q2, :]
                            kr = ("rt", q2)
                            lg = R[:, 0:16]
                            ex = R[:, 16:32]
                            pp = R[:, 32:56]
                            gs = R[:, 56:60]
                            gm = R[:, 60:64]
                            me = R[:, 64:80]
                            m2 = R[:, 80:96]
                            S.op("dve", lambda e, q2=q2, lg=lg: e.tensor_tensor(out=lg, in0=lgp[q2][:, :], in1=rb[:], op=ALU.add), r=[("lgp", q2), "rb"], w=[kr])
                            S.op("dve", lambda e, lg=lg, sm=sm: e.tensor_reduce(out=sm[:, 0:1], in_=lg, axis=AX.X, op=ALU.max), r=[kr], w=[kr])
                            S.op("dve", lambda e, sm=sm: e.tensor_scalar(out=sm[:, 1:2], in0=sm[:, 0:1], scalar1=-1.0, scalar2=None, op0=ALU.mult), r=[kr], w=[kr])
                            S.op("act", lambda e, lg=lg, ex=ex, sm=sm: e.activation(out=ex, in_=lg, func=AF.Exp, bias=sm[:, 1:2]), r=[kr], w=[kr])
                            e3 = ex.rearrange("p (g i) -> p g i", i=4)
                            p3 = pp.rearrange("p (g i) -> p g i", i=6)
                            S.op("dve", lambda e, e3=e3, p3=p3: e.tensor_tensor(out=p3[:, :, 0:3], in0=e3[:, :, 0:3], in1=e3[:, :, 1:4], op=ALU.add), r=[kr], w=[kr])
                            S.op("dve", lambda e, e3=e3, p3=p3: e.tensor_tensor(out=p3[:, :, 3:5], in0=e3[:, :, 0:2], in1=e3[:, :, 2:4], op=ALU.add), r=[kr], w=[kr])
                            S.op("dve", lambda e, e3=e3, p3=p3: e.tensor_tensor(out=p3[:, :, 5:6], in0=e3[:, :, 0:1], in1=e3[:, :, 3:4], op=ALU.add), r=[kr], w=[kr])
                            S.op("dve", lambda e, p3=p3, gs=gs: e.tensor_reduce(out=gs, in_=p3, axis=AX.X, op=ALU.max), r=[kr], w=[kr])
                            S.op("dve", lambda e, gs=gs, sm=sm: e.tensor_reduce(out=sm[:, 2:3], in_=gs, axis=AX.X, op=ALU.max), r=[kr], w=[kr])
                            S.op("dve", lambda e, gs=gs, gm=gm, sm=sm: e.tensor_scalar(out=gm, in0=gs, scalar1=sm[:, 2:3], scalar2=None, op0=ALU.is_ge), r=[kr], w=[kr])
                            S.op("dve", lambda e, e3=e3, gm=gm, me=me: e.tensor_tensor(out=me.rearrange("p (g i) -> p g i", i=4), in0=e3, in1=gm.unsqueeze(2).to_broadcast([128, 4, 4]), op=ALU.mult), r=[kr], w=[kr])
                            S.op("dve", lambda e, me=me, sm=sm: e.tensor_reduce(out=sm[:, 3:4], in_=me, axis=AX.X, op=ALU.max), r=[kr], w=[kr])
                            S.op("dve", lambda e, me=me, m2=m2, sm=sm: e.scalar_tensor_tensor(out=m2, in0=me, scalar=sm[:, 3:4], in1=me, op0=ALU.is_lt, op1=ALU.mult), r=[kr], w=[kr])
                            S.op("dve", lambda e, m2=m2, sm=sm: e.tensor_reduce(out=sm[:, 4:5], in_=m2, axis=AX.X, op=ALU.max), r=[kr], w=[kr])
                            S.op("dve", lambda e, me=me, m2=m2, sm=sm: e.scalar_tensor_tensor(out=m2, in0=me, scalar=sm[:, 4:5], in1=me, op0=ALU.is_ge, op1=ALU.mult), r=[kr], w=[kr])
                            S.op("dve", lambda e, sm=sm: e.tensor_tensor(out=sm[:, 5:6], in0=sm[:, 3:4], in1=sm[:, 4:5], op=ALU.add), r=[kr], w=[kr])
                            S.op("dve", lambda e, sm=sm: e.reciprocal(out=sm[:, 6:7], in_=sm[:, 5:6]), r=[kr], w=[kr])
                            S.op("dve", lambda e, m2=m2, sm=sm: e.tensor_scalar(out=m2, in0=m2, scalar1=sm[:, 6:7], scalar2=None, op0=ALU.mult), r=[kr], w=[kr])
                            if pend:
                                pend.pop()()

                            def fin(m2=m2, q2=q2, tk=tk, kr=kr, bi=bi):
                                S.op("pe", lambda e: e.transpose(out=gtp[q2][:, :], in_=m2, identity=ident[:]), r=[kr, "ident"], w=[("gtp", q2)])
                                S.op("act", lambda e: e.activation(out=gT[:, tk:tk + 128], in_=gtp[q2][:, :], func=AF.Copy), r=[("gtp", q2)], w=[("gT", bi)])
                            pend.append(fin)
                while pend:
                    pend.pop()()
            S.barrier()

        def moe(l, nblk, gT):
            with contextlib.ExitStack() as ph:
                sel = SBT(ph, "sel", [NE, NE, 128], BF16)
                g_hi = SBT(ph, "g_hi", [NE, TT], BF16)
                g_lo = SBT(ph, "g_lo", [NE, TT], BF16)
                with contextlib.ExitStack() as tph:
                    sel32 = SBT(tph, "sel32", [NE, NE, 128])
                    g_r = SBT(tph, "g_r", [NE, TT], F32)
                    S.dma("sp", sel32[:], sel_d[:, :, :], w=["sel32"])
                    S.op("dve", lambda e: e.tensor_copy(out=sel[:], in_=sel32[:]), r=["sel32"], w=["sel"])
                    S.op("dve", lambda e: e.tensor_copy(out=g_hi[:], in_=gT[:]), r=[("gT", bi_) for bi_ in range(nblk)], w=["g_hi"])
                    S.op("dve", lambda e: e.tensor_copy(out=g_r[:], in_=g_hi[:]), r=["g_hi"], w=["g_r"])
                    S.op("dve", lambda e: e.tensor_tensor(out=g_lo[:], in0=gT[:], in1=g_r[:], op=ALU.subtract), r=["g_r"] + [("gT", bi_) for bi_ in range(nblk)], w=["g_lo"])
                S.barrier()
                w13 = [SBT(ph, "w13_%d" % i, [128, 8, 2 * DE], BF16) for i in range(2)]
                w2b = [SBT(ph, "w2b_%d" % i, [128, 4, D], BF16) for i in range(2)]
                heT = [SBT(ph, "heT%d" % i, [128, 4, 512], BF16) for i in range(2)]
                Gsb = [SBT(ph, "Gsb%d" % i, [128, 512], F32) for i in range(2)]
                s1 = [SBT(ph, "s1_%d" % i, [128, 512], F32) for i in range(2)]
                t3 = [SBT(ph, "t3_%d" % i, [128, 512], F32) for i in range(2)]
                h1p = [PST(ph, "h1p%d" % i, [128, 512], F32) for i in range(2)]
                h3p = [PST(ph, "h3p%d" % i, [128, 512], F32) for i in range(2)]
                op_ = [PST(ph, "op%d" % i, [128, 512], F32) for i in range(2)]
                gp = PST(ph, "gp", [128, 512], F32)
                cnt = {"nf": 0, "no": 0}

                def load_w(ex):
                    e2 = ex % 2
                    S.dma("pool", w13[e2][:, :, 0:DE], moe_w1[l, ex].rearrange("(k p) f -> p k f", p=128), w=[("w13a", e2)])
                    S.dma("pool", w13[e2][:, :, DE:2 * DE], moe_w3[l, ex].rearrange("(k p) f -> p k f", p=128), w=[("w13b", e2)])
                    S.dma("pool", w2b[e2][:], moe_w2[l, ex].rearrange("(k p) d -> p k d", p=128), w=[("w2b", e2)])

                def up_fc(it, fc):
                    ex, bi, g2 = it
                    e2 = ex % 2
                    t0, n = blocks[bi]
                    f2 = cnt["nf"] % 2
                    cnt["nf"] += 1
                    for kk in range(8):
                        S.op("pe", lambda e, kk=kk: e.matmul(h1p[f2][:, 0:n], lhsT=w13[e2][:, kk, fc * 128:(fc + 1) * 128], rhs=hT[:, kk, t0:t0 + n], start=(kk == 0), stop=(kk == 7)),
                             r=[("w13a", e2), ("hT", bi)], w=[("h1p", f2)])
                    for kk in range(8):
                        S.op("pe", lambda e, kk=kk: e.matmul(h3p[f2][:, 0:n], lhsT=w13[e2][:, kk, DE + fc * 128:DE + (fc + 1) * 128], rhs=hT[:, kk, t0:t0 + n], start=(kk == 0), stop=(kk == 7)),
                             r=[("w13b", e2), ("hT", bi)], w=[("h3p", f2)])
                    S.op("act", lambda e: e.activation(out=s1[f2][:, 0:n], in_=h1p[f2][:, 0:n], func=AF.Silu), r=[("h1p", f2)], w=[("s1", f2)])
                    S.op("dve", lambda e: e.tensor_tensor(out=t3[f2][:, 0:n], in0=h3p[f2][:, 0:n], in1=Gsb[g2][:, 0:n], op=ALU.mult),
                         r=[("h3p", f2), ("Gsb", g2)], w=[("t3", f2)])
                    S.op("pool", lambda e: e.tensor_tensor(out=heT[g2][:, fc, 0:n], in0=s1[f2][:, 0:n], in1=t3[f2][:, 0:n], op=ALU.mult),
                         r=[("s1", f2), ("t3", f2)], w=[("heT", g2, fc)])

                def down_dc(it, dc):
                    ex, bi, g2 = it
                    e2 = ex % 2
                    t0, n = blocks[bi]
                    s = 0 if t0 < T else 1
                    o2 = cnt["no"] % 2
                    cnt["no"] += 1
                    for fc in range(4):
                        S.op("pe", lambda e, fc=fc: e.matmul(op_[o2][:, 0:n], lhsT=w2b[e2][:, fc, dc * 128:(dc + 1) * 128], rhs=heT[g2][:, fc, 0:n], start=(fc == 0), stop=(fc == 3)),
                             r=[("w2b", e2), ("heT", g2, fc)], w=[("op", o2)])
                    S.op("dve", lambda e: e.scalar_tensor_tensor(out=xT[:, dc, t0:t0 + n], in0=op_[o2][:, 0:n], scalar=modT[:, 40 + dc, s:s + 1], in1=xT[:, dc, t0:t0 + n], op0=ALU.mult, op1=ALU.add),
                         r=[("op", o2), "modT", ("xT", bi, dc)], w=[("xT", bi, dc)])

                items = [(ex, bi) for ex in range(NE) for bi in range(nblk)]
                load_w(0)
                prev = None
                for i, (ex, bi) in enumerate(items):
                    t0, n = blocks[bi]
                    g2 = i % 2
                    it = (ex, bi, g2)
                    S.op("pe", lambda e, ex=ex, t0=t0, n=n: e.matmul(gp[:, 0:n], lhsT=sel[:, ex, :], rhs=g_hi[:, t0:t0 + n], start=True, stop=False),
                         r=["sel", "g_hi"], w=["gp"])
                    S.op("pe", lambda e, ex=ex, t0=t0, n=n: e.matmul(gp[:, 0:n], lhsT=sel[:, ex, :], rhs=g_lo[:, t0:t0 + n], start=False, stop=True),
                         r=["sel", "g_lo"], w=["gp"])
                    S.op("act", lambda e, g2=g2, n=n: e.activation(out=Gsb[g2][:, 0:n], in_=gp[:, 0:n], func=AF.Copy), r=["gp"], w=[("Gsb", g2)])
                    for fc in range(4):
                        up_fc(it, fc)
                        if prev is not None:
                            down_dc(prev, 2 * fc)
                            down_dc(prev, 2 * fc + 1)
                    if bi == 0 and ex + 1 < NE:
                        load_w(ex + 1)
                    prev = it
                for dc in range(8):
                    down_dc(prev, dc)
            S.barrier()

        epsb = sb("epsb", [128, 1])
        S.op("dve", lambda e: e.memset(epsb[:], EPS), w=["epsb"])
        c1 = sb("c1", [128, 1])
        S.op("dve", lambda e: e.memset(c1[:], 1.0), w=["c1"])

        def load_win(ph, l, c0, ncols, name):
            wt = SBT(ph, name, [128, 8, ncols], BF16)
            S.dma("pool", wt[:], w_in[l, :, c0:c0 + ncols].rearrange("(k p) n -> p k n", p=128), w=[name])
            return wt

        def proj_out(ph, l, m, yTm, nblk):
            wo = SBT(ph, "wo", [128, 2, D], BF16)
            S.dma("pool", wo[:], w_out[l, m * 256:(m + 1) * 256, :].rearrange("(k p) n -> p k n", p=128), w=["wo"])
            pp = [PST(ph, "pop%d" % i, [128, 512], F32) for i in range(2)]
            no = 0
            for bi in range(nblk):
                t0, n = blocks[bi]
                s = 0 if t0 < T else 1
                for dc in range(8):
                    o2 = no % 2
                    no += 1
                    for j in range(2):
                        S.op("pe", lambda e, o2=o2, j=j, dc=dc, t0=t0, n=n: e.matmul(pp[o2][:, 0:n], lhsT=wo[:, j, dc * 128:(dc + 1) * 128], rhs=yTm[:, j, t0:t0 + n], start=(j == 0), stop=(j == 1)),
                             r=["wo", "yTm"], w=[("pop", o2)])
                    S.op("dve", lambda e, o2=o2, dc=dc, t0=t0, n=n, s=s: e.scalar_tensor_tensor(out=xT[:, dc, t0:t0 + n], in0=pp[o2][:, 0:n], scalar=modT[:, 16 + dc, s:s + 1], in1=xT[:, dc, t0:t0 + n], op0=ALU.mult, op1=ALU.add),
                         r=[("pop", o2), "modT", ("xT", bi, dc)], w=[("xT", bi, dc)])

        def mixer_lru(l, ctx_out):
            nblk = 5 if ctx_out else 4
            CO = T + 3
            with contextlib.ExitStack() as ph:
                yTm = SBT(ph, "yTmA", [128, 2, TT], BF16)
                with contextlib.ExitStack() as ph2:
                    win = load_win(ph2, l, 0, 512, "winA")
                    cw = SBT(ph2, "cw", [128, 2, 4])
                    cb = SBT(ph2, "cb", [128, 2])
                    gb = SBT(ph2, "gb", [128, 3, 2, 2])
                    wbd = SBT(ph2, "wbd", [128, 2, 2, 2, 128], BF16)
                    cl = SBT(ph2, "cl", [128, 2, 2, 2])
                    S.dma("sp", cw[:], lru_cw[l], w=["cw"])
                    S.dma("sp", cb[:], lru_cb[l], w=["cb"])
                    S.dma("sp", gb[:], lru_gb[l], w=["gb"])
                    S.dma("pool", wbd[:], lru_wbd[l], w=["wbd"])
                    S.op("act", lambda e: e.activation(out=cl[:, 0], in_=gb[:, 2], func=AF.Exp, scale=-1.0), r=["gb"], w=["cl"])
                    S.op("dve", lambda e: e.tensor_scalar(out=cl[:, 0], in0=cl[:, 0], scalar1=1.0, scalar2=None, op0=ALU.add), r=["cl"], w=["cl"])
                    S.op("act", lambda e: e.activation(out=cl[:, 0], in_=cl[:, 0], func=AF.Ln), r=["cl"], w=["cl"])
                    S.op("dve", lambda e: e.tensor_scalar(out=cl[:, 1], in0=cl[:, 0], scalar1=-16.0, scalar2=None, op0=ALU.mult), r=["cl"], w=["cl"])
                    S.op("dve", lambda e: e.tensor_scalar(out=cl[:, 0], in0=cl[:, 0], scalar1=-8.0, scalar2=None, op0=ALU.mult), r=["cl"], w=["cl"])
                    xpad = SBT(ph2, "xpad", [128, TT + 6])
                    u = SBT(ph2, "u", [128, TT])
                    ub = SBT(ph2, "ub", [128, TT], BF16)
                    rA = SBT(ph2, "rA", [128, TT])
                    iB = SBT(ph2, "iB", [128, TT])
                    sS = SBT(ph2, "sS", [128, TT])
                    hs = SBT(ph2, "hs", [128, TT])
                    gg = SBT(ph2, "gg", [128, TT])
                    zp = [PST(ph2, "zpA%d" % i, [128, 512], F32) for i in range(4)]
                    S.op("pool", lambda e: e.memset(xpad[:], 0.0), w=["xpad"])
                    nz = 0
                    for cc in range(2):
                        for bi in range(5):
                            t0, n = blocks[bi]
                            xo = (2 + t0) if t0 < T else (CO + 2)
                            z2 = nz % 4
                            nz += 1
                            for kk in range(8):
                                S.op("pe", lambda e, z2=z2, kk=kk, cc=cc, t0=t0, n=n: e.matmul(zp[z2][:, 0:n], lhsT=win[:, kk, cc * 128:(cc + 1) * 128], rhs=hT[:, kk, t0:t0 + n], start=(kk == 0), stop=(kk == 7)),
                                     r=["winA"], w=[("zp", z2)])
                            S.op("act", lambda e, z2=z2, xo=xo, n=n: e.activation(out=xpad[:, xo:xo + n], in_=zp[z2][:, 0:n], func=AF.Copy), r=[("zp", z2)], w=["xpad"])
                            if bi < nblk:
                                z2 = nz % 4
                                nz += 1
                                for kk in range(8):
                                    S.op("pe", lambda e, z2=z2, kk=kk, cc=cc, t0=t0, n=n: e.matmul(zp[z2][:, 0:n], lhsT=win[:, kk, 256 + cc * 128:256 + (cc + 1) * 128], rhs=hT[:, kk, t0:t0 + n], start=(kk == 0), stop=(kk == 7)),
                                         r=["winA"], w=[("zp", z2)])
                                S.op("act", lambda e, z2=z2, t0=t0, n=n: e.activation(out=gg[:, t0:t0 + n], in_=zp[z2][:, 0:n], func=AF.Gelu_apprx_tanh), r=[("zp", z2)], w=["gg"])
                        for (xo, uo, L) in ((0, 0, T), (CO, T, TC)):
                            S.op("dve", lambda e, xo=xo, uo=uo, L=L, cc=cc: e.tensor_scalar(out=u[:, uo:uo + L], in0=xpad[:, xo:xo + L], scalar1=cw[:, cc, 0:1], scalar2=cb[:, cc:cc + 1], op0=ALU.mult, op1=ALU.add),
                                 r=["xpad", "cw", "cb"], w=["u"])
                            for j in range(1, 4):
                                S.op("dve", lambda e, xo=xo, uo=uo, L=L, cc=cc, j=j: e.scalar_tensor_tensor(out=u[:, uo:uo + L], in0=xpad[:, xo + j:xo + j + L], scalar=cw[:, cc, j:j + 1], in1=u[:, uo:uo + L], op0=ALU.mult, op1=ALU.add),
                                     r=["xpad", "cw", "u"], w=["u"])
                        S.op("act", lambda e: e.activation(out=ub[:], in_=u[:], func=AF.Copy), r=["u"], w=["ub"])
                        if KCUT == 1:
                            continue
                        for d in range(2):
                            for bi in range(5):
                                t0, n = blocks[bi]
                                for gi, dst in ((0, rA), (1, iB)):
                                    z2 = nz % 4
                                    nz += 1
                                    S.op("pe", lambda e, z2=z2, gi=gi, d=d, cc=cc, t0=t0, n=n: e.matmul(zp[z2][:, 0:n], lhsT=wbd[:, gi, d, cc, :], rhs=ub[:, t0:t0 + n], start=True, stop=True),
                                         r=["wbd", "ub"], w=[("zp", z2)])
                                    S.op("act", lambda e, z2=z2, gi=gi, d=d, cc=cc, t0=t0, n=n, dst=dst: e.activation(out=dst[:, t0:t0 + n], in_=zp[z2][:, 0:n], func=AF.Sigmoid, bias=gb[:, gi, d, cc:cc + 1]),
                                         r=[("zp", z2), "gb"], w=["rA" if gi == 0 else "iB"])
                            if KCUT == 2:
                                continue
                            S.op("act", lambda e, d=d, cc=cc: e.activation(out=sS[:], in_=rA[:], func=AF.Exp, scale=cl[:, 1, d, cc:cc + 1]), r=["rA", "cl", "hs"], w=["sS"])
                            S.op("dve", lambda e: e.tensor_scalar(out=sS[:], in0=sS[:], scalar1=1.0, scalar2=-1.0, op0=ALU.min, op1=ALU.mult), r=["sS"], w=["sS"])
                            S.op("act", lambda e: e.activation(out=sS[:], in_=sS[:], func=AF.Sqrt, bias=c1[:, 0:1]), r=["sS", "c1"], w=["sS"])
                            S.op("act", lambda e, d=d, cc=cc: e.activation(out=rA[:], in_=rA[:], func=AF.Exp, scale=cl[:, 0, d, cc:cc + 1]), r=["rA", "cl"], w=["rA"])
                            S.op("dve", lambda e: e.tensor_tensor(out=iB[:], in0=iB[:], in1=sS[:], op=ALU.mult), r=["iB", "sS"], w=["iB"])
                            S.op("dve", lambda e: e.tensor_tensor(out=iB[:], in0=iB[:], in1=u[:], op=ALU.mult), r=["iB", "u"], w=["iB"])
                            if KCUT == 3:
                                continue
                            if d == 0:
                                S.op("dve", lambda e: e.tensor_tensor_scan(out=hs[:, T:TT], data0=rA[:, T:TT], data1=iB[:, T:TT], initial=0.0, op0=ALU.mult, op1=ALU.add),
                                     r=["rA", "iB"], w=["hs"])
                                S.op("dve", lambda e: e.tensor_tensor_scan(out=hs[:, 0:T], data0=rA[:, 0:T], data1=iB[:, 0:T], initial=hs[:, TT - 1:TT], op0=ALU.mult, op1=ALU.add),
                                     r=["rA", "iB", "hs"], w=["hs"])
                            elif KCUT != 4:
                                S.op("dve", lambda e: e.tensor_tensor_scan(out=sS[:, T:TT][:, ::-1], data0=rA[:, T:TT][:, ::-1], data1=iB[:, T:TT][:, ::-1], initial=0.0, op0=ALU.mult, op1=ALU.add),
                                     r=["rA", "iB", "sS"], w=["sS"])
                                S.op("dve", lambda e: e.tensor_tensor_scan(out=sS[:, 0:T][:, ::-1], data0=rA[:, 0:T][:, ::-1], data1=iB[:, 0:T][:, ::-1], initial=sS[:, T:T + 1], op0=ALU.mult, op1=ALU.add),
                                     r=["rA", "iB", "sS"], w=["sS"])
                                S.op("dve", lambda e: e.tensor_tensor(out=hs[:], in0=hs[:], in1=sS[:], op=ALU.add), r=["hs", "sS"], w=["hs"])
                        ny = TT if ctx_out else T
                        if KCUT in (4, 5):
                            continue
                        S.op("dve", lambda e, cc=cc, ny=ny: e.tensor_tensor(out=yTm[:, cc, 0:ny], in0=hs[:, 0:ny], in1=gg[:, 0:ny], op=ALU.mult), r=["hs", "gg"], w=["yTm"])
                S.barrier()
                dump("yTmA%d" % l, yTm[:])
                with contextlib.ExitStack() as ph3:
                    proj_out(ph3, l, 0, yTm, nblk)
            S.barrier()

        def mixer_sg(l, ctx_out):
            nblk = 5 if ctx_out else 4
            ntile = NT if ctx_out else T // 128
            with contextlib.ExitStack() as ph:
                yTm = SBT(ph, "yTmC", [128, 2, TT], BF16)
                with contextlib.ExitStack() as ph2:
                    win = load_win(ph2, l, 1280, 512, "winC")
                    wsp = SBT(ph2, "wsp", [128, 4, 128], BF16)
                    bsp = SBT(ph2, "bsp", [128, 4])
                    gbc = SBT(ph2, "gbc", [128, 256])
                    S.dma("pool", wsp[:], sg_wT[l], w=["wsp"])
                    S.dma("sp", bsp[:], sg_bT[l], w=["bsp"])
                    S.dma("sp", gbc[:], sg_g[l:l + 1, :].partition_broadcast(128), w=["gbc"])
                    zp = [PST(ph2, "zpC%d" % i, [128, 512], F32) for i in range(3)]
                    vmp = [PST(ph2, "vmp%d" % i, [128, 256], F32) for i in range(3)]
                    tpp = [PST(ph2, "tppC%d" % i, [128, 2, 128], BF16) for i in range(2)]
                    g = [SBT(ph2, "gC%d" % i, [128, 512]) for i in range(3)]
                    vb = [SBT(ph2, "vbC%d" % i, [128, 256], BF16) for i in range(3)]
                    ys = [SBT(ph2, "ysC%d" % i, [128, 256], BF16) for i in range(3)]
                    sm = SBT(ph2, "smC", [128, 3, 4])
                    junk = [SBT(ph2, "junkC%d" % i, [128, 256]) for i in range(3)]
                    def stA(t):
                        t2 = t % 3
                        cs = slice(t * 128, (t + 1) * 128)
                        for kk in range(8):
                            S.op("pe", lambda e, kk=kk: e.matmul(zp[t2][:, :], lhsT=hT[:, kk, cs], rhs=win[:, kk, :], start=(kk == 0), stop=(kk == 7)),
                                 r=["winC"], w=[("zp", t2)])
                        S.op("act", lambda e: e.activation(out=g[t2][:], in_=zp[t2][:, :], func=AF.Gelu_apprx_tanh), r=[("zp", t2)], w=[("g", t2)])
                        S.op("act", lambda e: e.activation(out=junk[t2][:], in_=g[t2][:, 256:512], func=AF.Square, accum_out=sm[:, t2, 0:1]), r=[("g", t2)], w=[("junk", t2), ("sm", t2)])
                        S.op("act", lambda e: e.activation(out=sm[:, t2, 1:2], in_=sm[:, t2, 0:1], func=AF.Sqrt, scale=1.0 / 256, bias=epsb[:, 0:1]), r=[("sm", t2), "epsb"], w=[("sm", t2)])
                        S.op("dve", lambda e: e.reciprocal(out=sm[:, t2, 2:3], in_=sm[:, t2, 1:2]), r=[("sm", t2)], w=[("sm", t2)])
                        S.op("dve", lambda e: e.scalar_tensor_tensor(out=vb[t2][:], in0=g[t2][:, 256:512], scalar=sm[:, t2, 2:3], in1=gbc[:], op0=ALU.mult, op1=ALU.mult),
                             r=[("g", t2), ("sm", t2), "gbc"], w=[("vb", t2)])

                    def stB(t):
                        t2 = t % 3
                        for h in range(4):
                            S.op("pe", lambda e, h=h: e.matmul(vmp[t2][:, h * 64:(h + 1) * 64], lhsT=wsp[:, h, :], rhs=vb[t2][:, h * 64:(h + 1) * 64], start=True, stop=True),
                                 r=["wsp", ("vb", t2)], w=[("vmp", t2)])
                        for h in range(4):
                            S.op("dve", lambda e, h=h: e.scalar_tensor_tensor(out=ys[t2][:, h * 64:(h + 1) * 64], in0=vmp[t2][:, h * 64:(h + 1) * 64], scalar=bsp[:, h:h + 1], in1=g[t2][:, h * 64:(h + 1) * 64], op0=ALU.add, op1=ALU.mult),
                                 r=[("vmp", t2), "bsp", ("g", t2)], w=[("ys", t2)])

                    def stC(t):
                        t2 = t % 3
                        tq = t % 2
                        cs = slice(t * 128, (t + 1) * 128)
                        for j in range(2):
                            S.op("pe", lambda e, j=j: e.transpose(out=tpp[tq][:, j, :], in_=ys[t2][:, j * 128:(j + 1) * 128], identity=identb[:]),
                                 r=[("ys", t2), "identb"], w=[("tpp", tq)])
                        S.op("act", lambda e: e.activation(out=yTm[:, :, cs], in_=tpp[tq][:], func=AF.Copy), r=[("tpp", tq)], w=["yTm"])

                    for t in range(ntile + 2):
                        if t < ntile:
                            stA(t)
                        if 0 <= t - 1 < ntile:
                            stB(t - 1)
                        if 0 <= t - 2 < ntile:
                            stC(t - 2)
                S.barrier()
                dump("yTmC%d" % l, yTm[:])
                with contextlib.ExitStack() as ph3:
                    proj_out(ph3, l, 2, yTm, nblk)
            S.barrier()

        def mixer_attn(l, ctx_out):
            nblk = 5 if ctx_out else 4
            nqt = NT if ctx_out else T // 128
            lam_init = 0.8 - 0.6 * math.exp(-0.3 * l)
            SC = 32.0 ** -0.5
            with contextlib.ExitStack() as ph:
                yTm = SBT(ph, "yTmB", [128, 2, TT], BF16)
                with contextlib.ExitStack() as ph1:
                    qT = SBT(ph1, "qT", [128, 2, TT], BF16)
                    kTm = [SBT(ph1, "kTm%d" % j, [128, 2, TT], BF16) for j in range(4)]
                    V = SBT(ph1, "V", [128, NT, 4, 65], BF16)
                    lamv = SBT(ph1, "lamv", [128, 4])
                    gsub = SBT(ph1, "gsub", [128, 64])
                    dl = SBT(ph1, "dl", [128, 128])
                    pr = SBT(ph1, "pr", [128, 64])
                    m4 = SBT(ph1, "m4", [128, 4])
                    S.dma("sp", dl[:], da_lam[l:l + 1, :].partition_broadcast(128), w=["dl"])
                    S.dma("sp", gsub[:], da_g[l:l + 1, :].partition_broadcast(128), w=["gsub"])
                    S.dma("sp", m4[:], m4_d[:, :], w=["m4"])
                    S.op("dve", lambda e: e.tensor_scalar(out=gsub[:], in0=gsub[:], scalar1=1.0 - lam_init, scalar2=None, op0=ALU.mult), r=["gsub"], w=["gsub"])
                    S.op("dve", lambda e: e.tensor_tensor(out=pr[:, 0:32], in0=dl[:, 0:32], in1=dl[:, 32:64], op=ALU.mult), r=["dl"], w=["pr"])
                    S.op("dve", lambda e: e.tensor_tensor(out=pr[:, 32:64], in0=dl[:, 64:96], in1=dl[:, 96:128], op=ALU.mult), r=["dl", "pr"], w=["pr"])
                    S.op("dve", lambda e: e.tensor_reduce(out=lamv[:, 0:2], in_=pr[:].rearrange("p (a b) -> p a b", b=32), axis=AX.X, op=ALU.add), r=["pr"], w=["lamv"])
                    S.op("act", lambda e: e.activation(out=lamv[:, 0:2], in_=lamv[:, 0:2], func=AF.Exp), r=["lamv"], w=["lamv"])
                    S.op("dve", lambda e: e.tensor_tensor(out=lamv[:, 2:3], in0=lamv[:, 0:1], in1=lamv[:, 1:2], op=ALU.subtract), r=["lamv"], w=["lamv"])
                    S.op("dve", lambda e: e.tensor_scalar(out=lamv[:, 3:4], in0=lamv[:, 2:3], scalar1=lam_init, scalar2=None, op0=ALU.add), r=["lamv"], w=["lamv"])
                    with contextlib.ExitStack() as ph2:
                        win = load_win(ph2, l, 512, 512, "winB")
                        winR = SBT(ph2, "winR", [128, 8, 512], BF16)
                        S.dma("pool", winR[:], w_inr[l].rearrange("(k p) n -> p k n", p=128), w=["winR"])
                        cs_t = [SBT(ph2, "cs%d" % i, [128, 2, 512]) for i in range(2)]
                        zp = [PST(ph2, "zpB%d" % i, [128, 512], F32) for i in range(4)]
                        t1 = [SBT(ph2, "t1_%d" % i, [128, 512]) for i in range(2)]
                        t2 = [SBT(ph2, "t2_%d" % i, [128, 512]) for i in range(2)]
                        S.op("pool", lambda e: e.memset(V[:].rearrange("p t h e -> p (t h) e")[:, :, 64:65], 1.0), w=["Vones"])
                        nz = 0
                        ni = 0
                        for c in range(2):
                            for which in range(2):
                                for bi in range(5):
                                    t0, n = blocks[bi]
                                    lat = t0 < T
                                    if which == 0 and not lat and not ctx_out:
                                        continue
                                    za = nz % 4
                                    nz += 1
                                    for kk in range(8):
                                        S.op("pe", lambda e, za=za, kk=kk, c=c, which=which, t0=t0, n=n: e.matmul(zp[za][:, 0:n], lhsT=win[:, kk, which * 256 + c * 128:which * 256 + (c + 1) * 128], rhs=hT[:, kk, t0:t0 + n], start=(kk == 0), stop=(kk == 7)),
                                             r=["winB"], w=[("zp", za)])
                                    if lat:
                                        zb = nz % 4
                                        nz += 1
                                        i2 = ni % 2
                                        ni += 1
                                        S.dma("sp", cs_t[i2][:, 0, :], cosT_d[:, t0:t0 + n], w=[("cs", i2)])
                                        S.dma("sp", cs_t[i2][:, 1, :], sinT_d[:, t0:t0 + n], w=[("cs", i2)])
                                        for kk in range(8):
                                            S.op("pe", lambda e, zb=zb, kk=kk, c=c, which=which, t0=t0, n=n: e.matmul(zp[zb][:, 0:n], lhsT=winR[:, kk, which * 256 + c * 128:which * 256 + (c + 1) * 128], rhs=hT[:, kk, t0:t0 + n], start=(kk == 0), stop=(kk == 7)),
                                                 r=["winR"], w=[("zp", zb)])
                                        S.op("dve", lambda e, za=za, i2=i2: e.tensor_tensor(out=t1[i2][:], in0=zp[za][:, :], in1=cs_t[i2][:, 0, :], op=ALU.mult), r=[("zp", za), ("cs", i2)], w=[("t1", i2)])
                                        S.op("dve", lambda e, zb=zb, i2=i2: e.tensor_tensor(out=t2[i2][:], in0=zp[zb][:, :], in1=cs_t[i2][:, 1, :], op=ALU.mult), r=[("zp", zb), ("cs", i2)], w=[("t2", i2)])
                                        if which == 0:
                                            S.op("dve", lambda e, i2=i2, c=c, t0=t0, n=n: e.tensor_tensor(out=qT[:, c, t0:t0 + n], in0=t1[i2][:], in1=t2[i2][:], op=ALU.add), r=[("t1", i2), ("t2", i2)], w=["qT"])
                                        else:
                                            S.op("dve", lambda e, i2=i2: e.tensor_tensor(out=t1[i2][:], in0=t1[i2][:], in1=t2[i2][:], op=ALU.add), r=[("t1", i2), ("t2", i2)], w=[("t1", i2)])
                                            for j in range(4):
                                                en = ("dve", "act", "pool", "act")[j]
                                                if en != "act":
                                                    S.op(en, lambda e, i2=i2, j=j, c=c, t0=t0, n=n: e.tensor_scalar(out=kTm[j][:, c, t0:t0 + n], in0=t1[i2][:], scalar1=m4[:, j:j + 1], scalar2=None, op0=ALU.mult), r=[("t1", i2), "m4"], w=["kTm"])
                                                else:
                                                    S.op("act", lambda e, i2=i2, j=j, c=c, t0=t0, n=n: e.activation(out=kTm[j][:, c, t0:t0 + n], in_=t1[i2][:], func=AF.Copy, scale=m4[:, j:j + 1]), r=[("t1", i2), "m4"], w=["kTm"])
                                    else:
                                        if which == 0:
                                            S.op("act", lambda e, za=za, c=c, t0=t0, n=n: e.activation(out=qT[:, c, t0:t0 + n], in_=zp[za][:, 0:n], func=AF.Copy), r=[("zp", za)], w=["qT"])
                                        else:
                                            for j in range(4):
                                                S.op("dve", lambda e, za=za, j=j, c=c, t0=t0, n=n: e.tensor_scalar(out=kTm[j][:, c, t0:t0 + n], in0=zp[za][:, 0:n], scalar1=m4[:, j:j + 1], scalar2=None, op0=ALU.mult), r=[("zp", za), "m4"], w=["kTm"])
                    S.barrier()
                    with contextlib.ExitStack() as ph2:
                        win = load_win(ph2, l, 1024, 256, "winBv")
                        zp = [PST(ph2, "zpBv%d" % i, [128, 512], F32) for i in range(4)]
                        nz = 0
                        for t in range(NT):
                            za = nz % 4
                            nz += 1
                            for kk in range(8):
                                S.op("pe", lambda e, za=za, kk=kk, t=t: e.matmul(zp[za][:, 0:256], lhsT=hT[:, kk, t * 128:(t + 1) * 128], rhs=win[:, kk, 0:256], start=(kk == 0), stop=(kk == 7)),
                                     r=["winBv"], w=[("zp", za)])
                            S.op("act", lambda e, za=za, t=t: e.activation(out=V[:, t, :, 0:64], in_=zp[za][:, 0:256].rearrange("p (h e) -> p h e", e=64), func=AF.Copy), r=[("zp", za)], w=["V"])
                    S.barrier()
                    with contextlib.ExitStack() as ph3:
                        ytm = SBT(ph3, "ytm", [128, NT, 256], BF16)
                        stp = [PST(ph3, "stp%d" % i, [128, 512], F32) for i in range(3)]
                        ob = [PST(ph3, "ob%d" % i, [128, 4, 65], F32) for i in range(4)]
                        pt = [SBT(ph3, "pt%d" % i, [128, 512], BF16) for i in range(3)]
                        ta = [SBT(ph3, "ta%d" % i, [128, 4, 64]) for i in range(2)]
                        tb = [SBT(ph3, "tb%d" % i, [128, 4, 64]) for i in range(2)]
                        rl = SBT(ph3, "rl", [128, 2, 4, 4])
                        st = {"step": 0, "it": 0}

                        def attend(h, q0, nq, kts):
                            c = h // 2
                            jb = (h % 2) * 2
                            nqs = nq // 128
                            it = st["it"] % 2
                            st["it"] += 1
                            O = [ob[it * 2 + n] for n in range(2)]
                            first = [True, True]
                            steps = [(ki, kt, n) for ki, kt in enumerate(kts) for n in range(2)]
                            bufs = {}

                            def emit_st(i):
                                ki, kt, n = steps[i]
                                i3 = st["step"] % 3
                                st["step"] += 1
                                bufs[i] = i3
                                S.op("pe", lambda e, i3=i3, n=n, kt=kt: e.matmul(stp[i3][:, 0:nq], lhsT=kTm[jb + n][:, c, kt * 128:(kt + 1) * 128], rhs=qT[:, c, q0:q0 + nq], start=True, stop=True),
                                     r=[], w=[("stp", i3)])
                                S.op("act", lambda e, i3=i3: e.activation(out=pt[i3][:, 0:nq], in_=stp[i3][:, 0:nq], func=AF.Exp, scale=SC), r=[("stp", i3)], w=[("pt", i3)])

                            def emit_pv(i):
                                ki, kt, n = steps[i]
                                i3 = bufs[i]
                                for qs in range(nqs):
                                    S.op("pe", lambda e, i3=i3, n=n, kt=kt, qs=qs, fs=first[n], ls=(ki == len(kts) - 1 and qs == nqs - 1): e.matmul(O[n][:, qs, :], lhsT=pt[i3][:, qs * 128:(qs + 1) * 128], rhs=V[:, kt, h, :], start=fs, stop=ls, skip_group_check=True),
                                         r=[("pt", i3)], w=[("O", it, n)])
                                    first[n] = False

                            LOOK = 2
                            for i in range(len(steps)):
                                emit_st(i)
                                if i >= LOOK:
                                    emit_pv(i - LOOK)
                            for i in range(max(0, len(steps) - LOOK), len(steps)):
                                emit_pv(i)
                            A_ = ta[it]
                            B_ = tb[it]
                            R_ = rl[:, it]
                            kr = ("rl", it)
                            for n in range(2):
                                S.op("dve", lambda e, n=n: e.reciprocal(out=R_[:, n, 0:nqs], in_=O[n][:, 0:nqs, 64]), r=[("O", it, n)], w=[kr])
                            S.op("dve", lambda e: e.tensor_scalar(out=R_[:, 1, 0:nqs], in0=R_[:, 1, 0:nqs], scalar1=lamv[:, 3:4], scalar2=None, op0=ALU.mult), r=[kr, "lamv"], w=[kr])
                            S.op("dve", lambda e: e.tensor_tensor(out=A_[:, 0:nqs, :], in0=O[1][:, 0:nqs, 0:64], in1=R_[:, 1, 0:nqs].unsqueeze(2).to_broadcast([128, nqs, 64]), op=ALU.mult), r=[("O", it, 1), kr], w=[("ta", it)])
                            S.op("dve", lambda e: e.tensor_tensor(out=B_[:, 0:nqs, :], in0=O[0][:, 0:nqs, 0:64], in1=R_[:, 0, 0:nqs].unsqueeze(2).to_broadcast([128, nqs, 64]), op=ALU.mult), r=[("O", it, 0), kr], w=[("tb", it)])
                            S.op("pool", lambda e: e.tensor_tensor(out=B_[:, 0:nqs, :], in0=B_[:, 0:nqs, :], in1=A_[:, 0:nqs, :], op=ALU.subtract), r=[("ta", it), ("tb", it)], w=[("tb", it)])
                            S.op("pool", lambda e: e.tensor_tensor(out=A_[:, 0:nqs, :], in0=B_[:, 0:nqs, :], in1=B_[:, 0:nqs, :], op=ALU.mult), r=[("tb", it)], w=[("ta", it)])
                            S.op("dve", lambda e: e.tensor_reduce(out=R_[:, 2, 0:nqs], in_=A_[:, 0:nqs, :], axis=AX.X, op=ALU.add), r=[("ta", it)], w=[kr])
                            S.op("act", lambda e: e.activation(out=R_[:, 3, 0:nqs], in_=R_[:, 2, 0:nqs], func=AF.Ln, scale=1.0 / 64, bias=epsb[:, 0:1]), r=[kr, "epsb"], w=[kr])
                            S.op("act", lambda e: e.activation(out=R_[:, 3, 0:nqs], in_=R_[:, 3, 0:nqs], func=AF.Exp, scale=-0.5), r=[kr], w=[kr])
                            S.op("dve", lambda e: e.tensor_tensor(out=A_[:, 0:nqs, :], in0=B_[:, 0:nqs, :], in1=R_[:, 3, 0:nqs].unsqueeze(2).to_broadcast([128, nqs, 64]), op=ALU.mult), r=[("tb", it), kr], w=[("ta", it)])
                            qt0 = q0 // 128
                            S.op("pool", lambda e: e.tensor_tensor(out=ytm[:, qt0:qt0 + nqs, h * 64:(h + 1) * 64], in0=A_[:, 0:nqs, :], in1=gsub[:].unsqueeze(1).to_broadcast([128, nqs, 64]), op=ALU.mult), r=[("ta", it), "gsub"], w=["ytm"])

                        for h in range(4):
                            for qb in range(4):
                                attend(h, qb * 512, 512, list(range(NT)))
                        if ctx_out:
                            for h in range(4):
                                attend(h, T, TC, [16, 17])
                        S.barrier()
                        with contextlib.ExitStack() as ph4:
                            tpp = [PST(ph4, "tppB%d" % i, [128, 2, 128], BF16) for i in range(1)]
                            for t in range(nqt):
                                t2_ = 0
                                for j in range(2):
                                    S.op("pe", lambda e, t=t, j=j: e.transpose(out=tpp[t2_][:, j, :], in_=ytm[:, t, j * 128:(j + 1) * 128], identity=identb[:]),
                                         r=["identb"], w=[("tpp", t2_)])
                                if t % 2 == 0:
                                    S.op("act", lambda e, t=t: e.activation(out=yTm[:, :, t * 128:(t + 1) * 128], in_=tpp[t2_][:], func=AF.Copy), r=[("tpp", t2_)], w=["yTm"])
                                else:
                                    S.op("dve", lambda e, t=t: e.tensor_copy(out=yTm[:, :, t * 128:(t + 1) * 128], in_=tpp[t2_][:]), r=[("tpp", t2_)], w=["yTm"])
                S.barrier()
                dump("yTmB%d" % l, yTm[:])
                with contextlib.ExitStack() as ph5:
                    proj_out(ph5, l, 1, yTm, nblk)
            S.barrier()

        def mixer_hgrn(l, ctx_out):
            nblk = 5 if ctx_out else 4
            nqt = NT if ctx_out else T // 128
            with contextlib.ExitStack() as ph:
                yTm = SBT(ph, "yTmD", [128, 2, TT], BF16)
                with contextlib.ExitStack() as ph1:
                    win = load_win(ph1, l, 1792, 1280, "winD")
                    osum = SBT(ph1, "osum", [128, nqt, 256])
                    sgall = SBT(ph1, "sgall", [128, nqt, 256], BF16)
                    tri = SBT(ph1, "tri", [128, 6, 128])
                    tokm = SBT(ph1, "tokm", [128, 2, 6])
                    S.dma("sp", tokm[:], tokm_d[:, :, :], w=["tokm"])
                    CI = SBT(ph1, "CI", [128, 2, 4])
                    vmf = osum[:, 0, :].rearrange("p (a b) -> p a b", a=2)
                    vm8 = SBT(ph1, "vm8", [128, 2, 4, 128], mybir.dt.uint8)
                    S.dma("sp", vmf, vmask_d[:, :, :], w=["vmf", ("osum", 0)])
                    for d in range(2):
                        S.op("dve", lambda e, d=d: e.tensor_copy(out=vm8[:, d], in_=vmf[:, d, :].unsqueeze(1).to_broadcast([128, 4, 128])), r=["vmf", ("osum", 0)], w=["vm8"])
                    gnb = SBT(ph1, "gnb", [128, 256])
                    S.dma("sp", tri[:], tri_d[:, :, :], w=["tri"])
                    S.dma("sp", CI[:], ci_d[:, :, :], w=["CI"])
                    S.dma("sp", gnb[:], hg_g[l:l + 1, :].partition_broadcast(128), w=["gnb"])
                    if l > 0:
                        lbb = SBT(ph1, "lbb", [128, 2, 2, 256])
                        with contextlib.ExitStack() as ph0:
                            lbr = SBT(ph0, "lbr", [128, 2, 2, 256])
                            S.dma("sp", lbr[:].rearrange("p a b w -> p (a b w)"), hg_lb_d[0:1, :].partition_broadcast(128), w=["lbr"])
                            S.op("dve", lambda e: e.tensor_tensor(out=lbb[:, 0], in0=lbr[:, :, 0, :], in1=lbr[:, :, 1, :], op=ALU.subtract), r=["lbr"], w=["lbb"])
                            S.op("act", lambda e: e.activation(out=lbb[:, 0], in_=lbb[:, 0], func=AF.Exp), r=["lbb"], w=["lbb"])
                            S.op("dve", lambda e: e.tensor_scalar(out=lbb[:, 0], in0=lbb[:, 0], scalar1=1.0, scalar2=None, op0=ALU.add), r=["lbb"], w=["lbb"])
                            S.op("dve", lambda e: e.reciprocal(out=lbb[:, 0], in_=lbb[:, 0]), r=["lbb"], w=["lbb"])
                            S.op("dve", lambda e: e.tensor_scalar(out=lbb[:, 1], in0=lbb[:, 0], scalar1=-1.0, scalar2=1.0, op0=ALU.mult, op1=ALU.add), r=["lbb"], w=["lbb"])
                            S.barrier()
                    zA = PST(ph1, "zA", [128, 512], F32)
                    zB = PST(ph1, "zB", [128, 512], F32)
                    bc = PST(ph1, "bc", [128, 2, 256], F32)
                    misc = PST(ph1, "misc", [128, 512], F32)
                    tp = PST(ph1, "tpD", [128, 8, 128], BF16)
                    attp = PST(ph1, "attp", [128, 4, 128], F32)
                    op2 = [PST(ph1, "oD%d" % i, [128, 256], F32) for i in range(2)]
                    decp = misc[:, 0:8].rearrange("p (c a) -> p c a", a=4)
                    Pps = misc[:, 128:384].rearrange("p (c h v) -> p c h v", h=2, v=64)
                    S32 = [[SBT(ph1, "S32_%d%d" % (d, k_), [128, 2, 64]) for k_ in range(2)] for d in range(2)]
                    scur = [0, 0]
                    Sbf = [SBT(ph1, "Sbf_%d" % d, [128, 2, 2, 2, 64], BF16) for d in range(2)]
                    for t in range(nqt):
                        S.op("pool", lambda e, t=t: e.memset(osum[:, t, :], 0.0), w=[("osum", t)])
                    for d in range(2):
                        for k_ in range(2):
                            S.op("pool", lambda e, d=d, k_=k_: e.memset(S32[d][k_][:], 0.0), w=[("S32", d, k_)])
                        S.op("pool", lambda e, d=d: e.memset(Sbf[d][:], 0.0), w=[("Sbf", d, xy, hh) for xy in range(2) for hh in range(2)])
                    W = {}
                    for d in range(2):
                        for b in range(1):
                            W[d, b] = dict(
                                kk=SBT(ph1, "kk_%d%d" % (d, b), [128, 256]),
                                ex=SBT(ph1, "ex_%d%d" % (d, b), [128, 3, 256]), dec=SBT(ph1, "dec_%d%d" % (d, b), [128, 2, 4]),
                                q32=SBT(ph1, "q32_%d%d" % (d, b), [128, 256]),
                                qt=SBT(ph1, "qt_%d%d" % (d, b), [128, 4, 256], BF16), kh=SBT(ph1, "kh_%d%d" % (d, b), [128, 2, 256], BF16), kTp=SBT(ph1, "kTp_%d%d" % (d, b), [128, 2, 4, 128], BF16),
                                v=[SBT(ph1, "v_%d%d_%d" % (d, b, k_), [128, 256], BF16) for k_ in range(2)], qkT=SBT(ph1, "qkT_%d%d" % (d, b), [128, 4, 128], BF16),
                                attm=SBT(ph1, "attm_%d%d" % (d, b), [128, 4, 128], BF16),
                            )
                            W[d, b]["f"] = W[d, b]["ex"][:, 0, :]
                            W[d, b]["lf"] = W[d, b]["ex"][:, 2, :]
                            if d == 0:
                                W[d, b]["gtmp"] = SBT(ph1, "gtmp_%d%d" % (d, b), [128, 256])

                    for key_ in W:
                        S.op("pool", lambda e, key_=key_: e.memset(W[key_]["attm"][:], 0.0), w=[("attm",) + key_])
                        S.op("pool", lambda e, key_=key_: e.memset(W[key_]["kTp"][:], 0.0), w=[("kTp",) + key_])

                    def prep(t, d, b, need_out, it):
                        w = W[d, b]
                        vv = w["v"][it % 2]
                        kv = ("v", d, it % 2)
                        kf = ("ex", d, 0)
                        klf = ("ex", d, 2)
                        kkk = ("kk", d, b)
                        cs = slice(t * 128, (t + 1) * 128)
                        groups = [(zA, 0, 0), (zA, 256, 256 + d * 256), (zB, 0, 768)]
                        if d == 0 and need_out:
                            groups.append((zB, 256, 1024))
                        for (zt, zo, wc) in groups:
                            for kk_ in range(8):
                                S.op("pe", lambda e, zt=zt, zo=zo, wc=wc, kk_=kk_: e.matmul(zt[:, zo:zo + 256], lhsT=hT[:, kk_, cs], rhs=win[:, kk_, wc:wc + 256], start=(kk_ == 0), stop=(kk_ == 7)),
                                     r=["winD"], w=["zA" if zt is zA else "zB"])
                        S.op("act", lambda e: e.activation(out=w["f"][:], in_=zA[:, 256:512], func=AF.Exp, scale=-1.0), r=["zA"], w=[kf])
                        S.op("act", lambda e: e.activation(out=w["q32"][:], in_=zA[:, 0:256], func=AF.Copy), r=["zA"], w=[("q32", d, b)])
                        S.op("act", lambda e: e.activation(out=vv[:], in_=zB[:, 0:256], func=AF.Copy), r=["zB"], w=[kv])
                        if d == 0 and need_out:
                            S.op("act", lambda e: e.activation(out=w["gtmp"][:], in_=zB[:, 256:512], func=AF.Exp, scale=-1.0), r=["zB"], w=[("gtmp", d, b)])
                            S.op("act", lambda e: e.activation(out=w["gtmp"][:], in_=w["gtmp"][:], func=AF.Ln, bias=c1[:, 0:1]), r=[("gtmp", d, b), "c1"], w=[("gtmp", d, b)])
                            S.op("act", lambda e: e.activation(out=w["gtmp"][:], in_=w["gtmp"][:], func=AF.Exp, scale=-1.0), r=[("gtmp", d, b)], w=[("gtmp", d, b)])
                            S.op("dve", lambda e: e.tensor_tensor(out=sgall[:, t, :], in0=zB[:, 256:512], in1=w["gtmp"][:], op=ALU.mult), r=["zB", ("gtmp", d, b)], w=["sgall"])
                        S.seg()
                        S.op("act", lambda e: e.activation(out=w["f"][:], in_=w["f"][:], func=AF.Ln, bias=c1[:, 0:1]), r=[kf, "c1"], w=[kf])
                        S.op("act", lambda e: e.activation(out=w["f"][:], in_=w["f"][:], func=AF.Exp, scale=-1.0), r=[kf], w=[kf])
                        if l > 0:
                            S.op("dve", lambda e: e.tensor_tensor(out=w["f"][:], in0=w["f"][:], in1=lbb[:, 1, d, :], op=ALU.mult), r=[kf, "lbb"], w=[kf])
                            S.op("dve", lambda e: e.tensor_tensor(out=w["f"][:], in0=w["f"][:], in1=lbb[:, 0, d, :], op=ALU.add), r=[kf, "lbb"], w=[kf])
                        S.op("act", lambda e: e.activation(out=w["lf"][:], in_=w["f"][:], func=AF.Ln), r=[kf], w=[klf])
                        S.op("pool", lambda e: e.tensor_scalar(out=w["kk"][:], in0=w["f"][:], scalar1=-1.0, scalar2=1.0, op0=ALU.mult, op1=ALU.add), r=[kf], w=[kkk])
                        S.seg()
                        for j in range(2):
                            S.op("pe", lambda e, j=j: e.matmul(bc[:, j, :], lhsT=tri[:, 3 * d + j, :], rhs=w["lf"][:], start=True, stop=True), r=[klf, "tri"], w=["bc"])
                        for c in range(2):
                            S.op("pe", lambda e, c=c: e.matmul(decp[:, c, :], lhsT=w["lf"][:, c * 128:(c + 1) * 128], rhs=CI[:, d, :], start=True, stop=True), r=[klf, "CI"], w=["decp"])
                        S.op("pe", lambda e: e.matmul(zB[:, 0:256], lhsT=tri[:, 3 * d + 2, :], rhs=w["lf"][:], start=True, stop=True), r=[klf, "tri"], w=["zB"])
                        S.op("act", lambda e: e.activation(out=w["dec"][:], in_=decp, func=AF.Exp), r=["decp"], w=[("dec", d, b)])
                        S.op("act", lambda e: e.activation(out=w["ex"][:, 0, :], in_=bc[:, 0, :], func=AF.Exp), r=["bc", ("ex", d, 0)], w=[("ex", d, 0)])
                        S.op("act", lambda e: e.activation(out=w["ex"][:, 1, :], in_=bc[:, 0, :], func=AF.Exp, scale=-1.0), r=["bc"], w=[("ex", d, 1)])
                        S.op("act", lambda e: e.activation(out=w["ex"][:, 2, :], in_=bc[:, 1, :], func=AF.Exp), r=["bc"], w=[("ex", d, 2)])
                        S.op("dve", lambda e: e.scalar_tensor_tensor(out=w["qt"][:, 0, :], in0=w["q32"][:], scalar=tokm[:, d, 2:3], in1=w["ex"][:, 0, :], op0=ALU.mult, op1=ALU.mult), r=[("q32", d, b), ("ex", d, 0), "tokm"], w=[("qt", d, b)])
                        S.op("dve", lambda e: e.scalar_tensor_tensor(out=w["qt"][:, 2, :], in0=w["kk"][:], scalar=tokm[:, d, 0:1], in1=w["ex"][:, 1, :], op0=ALU.mult, op1=ALU.mult), r=[kkk, ("ex", d, 1), "tokm"], w=[("qt", d, b)])
                        S.op("dve", lambda e: e.scalar_tensor_tensor(out=w["qt"][:, 1, :], in0=w["q32"][:], scalar=tokm[:, d, 3:4], in1=w["ex"][:, 2, :], op0=ALU.mult, op1=ALU.mult), r=[("q32", d, b), ("ex", d, 2), "tokm"], w=[("qt", d, b)])
                        S.op("act", lambda e: e.activation(out=w["ex"][:, 0, :], in_=bc[:, 1, :], func=AF.Exp, scale=-1.0), r=["bc"], w=[("ex", d, 0)])
                        S.op("act", lambda e: e.activation(out=w["ex"][:, 1, :], in_=zB[:, 0:256], func=AF.Exp), r=["zB"], w=[("ex", d, 1)])
                        S.op("dve", lambda e: e.tensor_tensor(out=w["qt"][:, 3, :], in0=w["kk"][:], in1=w["ex"][:, 0, :], op=ALU.mult), r=[kkk, ("ex", d, 0)], w=[("qt", d, b)])
                        for a_ in range(2):
                            S.op("dve", lambda e, a_=a_: e.scalar_tensor_tensor(out=w["kh"][:, a_, :], in0=w["kk"][:], scalar=tokm[:, d, 4 + a_:5 + a_], in1=w["ex"][:, 1, :], op0=ALU.mult, op1=ALU.mult), r=[kkk, ("ex", d, 1), "tokm"], w=[("kh", d, b)])
                        S.seg()
                        for j in range(8):
                            S.op("pe", lambda e, j=j: e.transpose(out=tp[:, j, :], in_=w["qt"][:, j // 2, (j % 2) * 128:(j % 2 + 1) * 128], identity=identb[:]), r=[("qt", d, b), "identb"], w=["tp"])
                        S.op("act", lambda e: e.activation(out=w["qkT"][:], in_=tp[:, 0:4, :], func=AF.Copy), r=["tp"], w=[("qkT", d, b)])
                        S.op("act", lambda e: e.activation(out=w["kTp"][0:64, 0, :, :], in_=tp[0:64, 4:8, :], func=AF.Copy), r=["tp"], w=[("kTp", d, b)])
                        S.op("dve", lambda e: e.tensor_copy(out=w["kTp"][64:128, 1, :, :], in_=tp[64:128, 4:8, :]), r=["tp"], w=[("kTp", d, b)])

                    def chain(t, d, b, need_out, it):
                        w = W[d, b]
                        vv = w["v"][it % 2]
                        kv = ("v", d, it % 2)
                        o_ = op2[d]
                        if need_out:
                            for h in range(4):
                                c, hb = h // 2, 64 * (h % 2)
                                hh = h % 2
                                S.op("pe", lambda e, h=h, c=c, hh=hh: e.matmul(attp[:, h, :], lhsT=w["kTp"][:, hh, c, :], rhs=w["qkT"][:, c, :], start=True, stop=False),
                                     r=[("qkT", d, b), ("kTp", d, b)], w=["attp"])
                                S.op("pe", lambda e, h=h, c=c, hh=hh: e.matmul(attp[:, h, :], lhsT=w["kTp"][:, hh, 2 + c, :], rhs=w["qkT"][:, 2 + c, :], start=False, stop=True),
                                     r=[("qkT", d, b), ("kTp", d, b)], w=["attp"])
                            S.op("dve", lambda e: e.copy_predicated(out=w["attm"][:], mask=vm8[:, d], data=attp[:]), r=["attp", "vm8"], w=[("attm", d, b)])
                            S.seg()
                            for h in range(4):
                                S.op("pe", lambda e, h=h: e.matmul(o_[:, h * 64:(h + 1) * 64], lhsT=w["attm"][:, h, :], rhs=vv[:, h * 64:(h + 1) * 64], start=(h == 0), stop=False, skip_group_check=True),
                                     r=[("attm", d, b), kv], w=[("oD", d)])
                        for a in ((0, 1) if d == 0 else (1, 0)):
                            cur = scur[d]
                            Sc = S32[d][cur]
                            Sn = S32[d][1 - cur]
                            scur[d] = 1 - cur
                            if need_out:
                                for hh in range(2):
                                    hb = 64 * hh
                                    S.op("pool", lambda e, hh=hh, hb=hb, Sc=Sc: e.tensor_copy(out=Sbf[d][hb:hb + 64, 0, :, hh, :], in_=Sc[hb:hb + 64, :, :]), r=[("S32", d, cur)], w=[("Sbf", d, 0, hh)])
                                    S.op("dve", lambda e, hh=hh, hb=hb, a=a, Sc=Sc: e.tensor_tensor(out=Sbf[d][hb:hb + 64, 1, :, hh, :], in0=Sc[hb:hb + 64, :, :], in1=w["dec"][hb:hb + 64, :, 2 + a:3 + a].to_broadcast([64, 2, 64]), op=ALU.mult), r=[("S32", d, cur), ("dec", d, b)], w=[("Sbf", d, 1, hh)])
                                S.seg()
                                for h in range(4):
                                    c, hh = h // 2, h % 2
                                    for xy in range(2):
                                        S.op("pe", lambda e, h=h, c=c, hh=hh, a=a, xy=xy: e.matmul(o_[a * 64:(a + 1) * 64, h * 64:(h + 1) * 64], lhsT=w["qkT"][:, xy * 2 + c, a * 64:(a + 1) * 64], rhs=Sbf[d][:, xy, c, hh, :], start=False, stop=True, skip_group_check=True),
                                             r=[("qkT", d, b), ("Sbf", d, xy, hh)], w=[("oD", d)])
                            for c in range(2):
                                for hh in range(2):
                                    h = 2 * c + hh
                                    S.op("pe", lambda e, h=h, c=c, hh=hh, a=a: e.matmul(Pps[:, c, hh, :], lhsT=w["kh"][:, a, c * 128:(c + 1) * 128], rhs=vv[:, h * 64:(h + 1) * 64], start=True, stop=True, skip_group_check=True),
                                         r=[("kh", d, b), kv], w=["Pps"])
                            for c in range(2):
                                for hh in range(2):
                                    hb = 64 * hh
                                    S.op("dve", lambda e, c=c, hh=hh, hb=hb, a=a, Sc=Sc, Sn=Sn: e.scalar_tensor_tensor(out=Sn[hb:hb + 64, c, :], in0=Sc[hb:hb + 64, c, :], scalar=w["dec"][hb:hb + 64, c, a:a + 1], in1=Pps[hb:hb + 64, c, hh, :], op0=ALU.mult, op1=ALU.add),
                                         r=[("S32", d, cur), ("dec", d, b), "Pps"], w=[("S32", d, 1 - cur)])
                            S.seg()
                        if need_out:
                            S.op("dve", lambda e: e.tensor_tensor(out=osum[:, t, :], in0=osum[:, t, :], in1=o_[:, :], op=ALU.add), r=[("oD", d), ("osum", t)], w=[("osum", t)])

                    order_f = [16, 17] + list(range(16))
                    order_b = [17, 16] + list(range(15, -1, -1))
                    def rec_prep(i):
                        return [S.split(S.record(lambda d=d, t=t: prep(t, d, 0, ctx_out or t < 16, i))) for d, t in ((0, order_f[i]), (1, order_b[i]))]

                    S.emit_segs(rec_prep(0))
                    for i in range(NT):
                        pair = ((0, order_f[i]), (1, order_b[i]))
                        nxt = rec_prep(i + 1) if i + 1 < NT else None
                        if nxt is not None:
                            S.emit_segs([p[:1] for p in nxt])
                        S.emit_interleaved([S.record(lambda d=d, t=t: chain(t, d, 0, ctx_out or t < 16, i)) for d, t in pair])
                        if nxt is not None:
                            S.emit_segs([p[1:] for p in nxt])
                    S.barrier()
                    with contextlib.ExitStack() as ph2:
                        sq = [W[0, 0]["ex"][:, i, :] for i in range(2)]
                        ssD = SBT(ph2, "ssD", [128, 2, 8])
                        yv = [W[0, 0]["qt"][:, i, :] for i in range(2)]
                        for t in range(nqt):
                            t2 = t % 2
                            o3 = osum[:, t, :].rearrange("p (h e) -> p h e", e=64)
                            S.op("pool", lambda e, t2=t2, t=t: e.tensor_tensor(out=sq[t2], in0=osum[:, t, :], in1=osum[:, t, :], op=ALU.mult), r=[], w=[("sqD", t2)])
                            S.op("dve", lambda e, t2=t2: e.tensor_reduce(out=ssD[:, t2, 0:4], in_=sq[t2].rearrange("p (h e) -> p h e", e=64), axis=AX.X, op=ALU.add), r=[("sqD", t2)], w=[("ssD", t2)])
                            S.op("act", lambda e, t2=t2: e.activation(out=ssD[:, t2, 4:8], in_=ssD[:, t2, 0:4], func=AF.Sqrt, scale=1.0 / 64, bias=epsb[:, 0:1]), r=[("ssD", t2), "epsb"], w=[("ssD", t2)])
                            S.op("dve", lambda e, t2=t2: e.reciprocal(out=ssD[:, t2, 4:8], in_=ssD[:, t2, 4:8]), r=[("ssD", t2)], w=[("ssD", t2)])
                            S.op("dve", lambda e, t2=t2, o3=o3: e.tensor_tensor(out=sq[t2].rearrange("p (h e) -> p h e", e=64), in0=o3, in1=ssD[:, t2, 4:8].unsqueeze(2).to_broadcast([128, 4, 64]), op=ALU.mult), r=[("ssD", t2), ("sqD", t2)], w=[("sqD", t2)])
                            S.op("dve", lambda e, t2=t2: e.tensor_tensor(out=sq[t2], in0=sq[t2], in1=gnb[:], op=ALU.mult), r=[("sqD", t2), "gnb"], w=[("sqD", t2)])
                            S.op("pool", lambda e, t2=t2, t=t: e.tensor_tensor(out=yv[t2], in0=sq[t2], in1=sgall[:, t, :], op=ALU.mult), r=[("sqD", t2)], w=[("yvD", t2)])
                            for j in range(2):
                                S.op("pe", lambda e, t2=t2, j=j: e.transpose(out=tp[:, j, :], in_=yv[t2][:, j * 128:(j + 1) * 128], identity=identb[:]), r=[("yvD", t2), "identb"], w=["tp"])
                            S.op("act", lambda e, t=t: e.activation(out=yTm[:, :, t * 128:(t + 1) * 128], in_=tp[:, 0:2, :], func=AF.Copy), r=["tp"], w=["yTm"])
                S.barrier()
                dump("yTmD%d" % l, yTm[:])
                with contextlib.ExitStack() as ph5:
                    proj_out(ph5, l, 3, yTm, nblk)
            S.barrier()

        def dump(name, ap_sb, shape_note=None):
            if name in dbg_d:
                S.barrier()
                S.dma("sp", dbg_d[name], ap_sb, w=["dbgout"])
                S.barrier()


        for l in range(layers):
            ctx_out = l < DEPTH - 1
            modT = modTs[l]
            gsc = gscs[l]
            dump("modT%d" % l, modT[:])
            rmsnorm_to_hT(0, 5, router=False)
            dump("hT%d" % l, None)
            if stop_after == ("norm1", l):
                break
            if "A" in mixers:
                mixer_lru(l, ctx_out)
            if "B" in mixers:
                mixer_attn(l, ctx_out)
            if "C" in mixers:
                mixer_sg(l, ctx_out)
            if "D" in mixers:
                mixer_hgrn(l, ctx_out)
            if skip_moe:
                continue
            with contextlib.ExitStack() as phg:
                gT = SBT(phg, "gT", [NE, TT])
                rmsnorm_to_hT(1, 5 if ctx_out else 4, router=True, gT=gT)
                dump("gT%d" % l, gT[:])
                moe(l, 5 if ctx_out else 4, gT)
            dump("xT%d" % l, xT[:])

        with contextlib.ExitStack() as ph:
            sq = [SBT(ph, "fsq%d" % i, [128, 8, 512], BF16) for i in range(2)]
            rs = [SBT(ph, "frs%d" % i, [128, 512], F32) for i in range(2)]
            yb = [SBT(ph, "fyb%d" % i, [128, 8, 512], F32) for i in range(2)]
            ost = [SBT(ph, "ost%d" % i, [128, D], F32) for i in range(2)]
            ssp = [PST(ph, "fssp%d" % i, [128, 512], F32) for i in range(2)]
            tp = [PST(ph, "ftp%d" % i, [128, 4, 128], F32) for i in range(2)]
            n_tp = 0
            for bi in range(4):
                t0, n = blocks[bi]
                b2 = bi % 2
                S.op("act", lambda e, b2=b2, t0=t0, n=n: e.activation(out=sq[b2][:, :, 0:n], in_=xT[:, :, t0:t0 + n], func=AF.Square), r=[("xT", bi)], w=[("sq", b2)])
                for c in range(8):
                    S.op("pe", lambda e, b2=b2, c=c, n=n: e.matmul(ssp[b2][:, 0:n], lhsT=ones_b[:], rhs=sq[b2][:, c, 0:n], start=(c == 0), stop=(c == 7)),
                         r=[("sq", b2), "ones_b"], w=[("ssp", b2)])
                S.op("act", lambda e, b2=b2, n=n: e.activation(out=rs[b2][:, 0:n], in_=ssp[b2][:, 0:n], func=AF.Ln, scale=1.0 / D, bias=epsb[:, 0:1]),
                     r=[("ssp", b2), "epsb"], w=[("rs", b2)])
                S.op("act", lambda e, b2=b2, n=n: e.activation(out=rs[b2][:, 0:n], in_=rs[b2][:, 0:n], func=AF.Exp, scale=-0.5), r=[("rs", b2)], w=[("rs", b2)])
                for c in range(8):
                    S.op("dve", lambda e, c=c, t0=t0, n=n, b2=b2: e.scalar_tensor_tensor(out=yb[b2][:, c, 0:n], in0=xT[:, c, t0:t0 + n], scalar=gvec[:, 2, c:c + 1], in1=rs[b2][:, 0:n], op0=ALU.mult, op1=ALU.mult),
                         r=[("xT", bi), "gvec2", ("rs", b2)], w=[("yb", b2, c)])
                for ti in range(4):
                    tk = t0 + ti * 128
                    o2 = (tk // 128) % 2
                    for half in range(2):
                        p = tp[n_tp % 2]
                        for j in range(4):
                            c = half * 4 + j
                            S.op("pe", lambda e, p=p, j=j, c=c, b2=b2, ti=ti: e.transpose(out=p[:, j, :], in_=yb[b2][:, c, ti * 128:(ti + 1) * 128], identity=ident[:]),
                                 r=[("yb", b2, c), "ident"], w=[("ftp", n_tp % 2)])
                        if n_tp % 2 == 0:
                            S.op("act", lambda e, p=p, o2=o2, half=half: e.activation(out=ost[o2][:, half * 512:(half + 1) * 512], in_=p[:].rearrange("p a b -> p (a b)"), func=AF.Copy),
                                 r=[("ftp", n_tp % 2)], w=[("ost", o2)])
                        else:
                            S.op("dve", lambda e, p=p, o2=o2, half=half: e.tensor_copy(out=ost[o2][:, half * 512:(half + 1) * 512], in_=p[:].rearrange("p a b -> p (a b)")),
                                 r=[("ftp", n_tp % 2)], w=[("ost", o2)])
                        n_tp += 1
                    S.dma("sp", out_d[tk:tk + 128, :], ost[o2][:], r=[("ost", o2)])
            S.finish("sp")
    k.n_inst = S.n_inst
    return nc


_PERM = _perm_rope()


def _prep_shared(inp):
    f = lambda a: np.ascontiguousarray(np.asarray(a, dtype=np.float32))
    sh = {}
    sh["w_ada"] = f(inp["w_ada"])
    sh["b_adaT"] = f(np.asarray(inp["b_ada"]).reshape(DEPTH, 48, 128).transpose(0, 2, 1))
    sh["n1g"] = f(np.asarray(inp["norm1_g"]).reshape(DEPTH, 8, 128).transpose(0, 2, 1))
    sh["n2g"] = f(np.asarray(inp["norm2_g"]).reshape(DEPTH, 8, 128).transpose(0, 2, 1))
    sh["fng"] = f(np.asarray(inp["final_norm_g"]).reshape(8, 128).T)
    w_in = np.asarray(inp["w_in"])
    sh["w_in"] = f(w_in)
    qk = w_in[:, :, 512:1024]
    sh["w_inr"] = f(np.concatenate([qk[:, :, 0:256][:, :, _PERM], qk[:, :, 256:512][:, :, _PERM]], axis=2))
    sh["w_out"] = f(inp["w_out"])
    sh["router_w"] = f(np.asarray(inp["router_w"]).reshape(8, 128, NE).transpose(1, 0, 2))
    sh["router_b"] = f(np.asarray(inp["router_b"]).reshape(1, NE))
    sh["moe_w1"] = f(inp["moe_w1"])
    sh["moe_w3"] = f(inp["moe_w3"])
    sh["moe_w2"] = f(inp["moe_w2"])
    sh["lru_cw"] = f(np.asarray(inp["lru_conv_w"]).reshape(DEPTH, 4, 2, 128).transpose(0, 3, 2, 1))
    sh["lru_cb"] = f(np.asarray(inp["lru_conv_b"]).reshape(DEPTH, 2, 128).transpose(0, 2, 1))
    gbs = np.stack([np.asarray(inp["lru_br"]), np.asarray(inp["lru_bi"]), np.asarray(inp["lru_lam"])], axis=1)
    sh["lru_gb"] = f(gbs.reshape(DEPTH, 3, 2, 2, 128).transpose(0, 4, 1, 2, 3))
    wbd = np.zeros((DEPTH, 128, 2, 2, 2, 128), np.float32)
    for gi, nm in enumerate(("lru_wr", "lru_wi")):
        w = np.asarray(inp[nm])
        for d in range(2):
            for hh in range(4):
                cc, h2 = hh // 2, hh % 2
                wbd[:, h2 * 64:(h2 + 1) * 64, gi, d, cc, h2 * 64:(h2 + 1) * 64] = w[:, d, hh]
    sh["lru_wbd"] = wbd
    sh["sg_wT"] = f(np.asarray(inp["sg_w"]).transpose(0, 3, 1, 2))
    sh["sg_bT"] = f(np.asarray(inp["sg_b"]).transpose(0, 2, 1))
    sh["sg_g"] = f(inp["sg_norm_g"])
    sh["da_lam"] = f(np.asarray(inp["da_lam"]).reshape(DEPTH, 128))
    sh["da_g"] = f(inp["da_subln_g"])
    sh["m4"] = f((np.arange(128)[:, None] // 32) == np.arange(4)[None, :])
    cosT, sinT = _rope_tables()
    sh["cosT"] = f(cosT)
    sh["sinT"] = f(sinT)
    sI = np.arange(128)[:, None]
    tI = np.arange(128)[None, :]
    same = (sI // 64) == (tI // 64)
    sl = sI % 64
    tl = tI % 64
    F_ = lambda m: m.astype(np.float32)
    sh["tri"] = f(np.stack([
        F_(same & (tl <= 31) & (sI <= tI)), F_(same & (sI <= tI)) - F_(same & (sl <= 31)), F_(same & (sI > tI)),
        F_(same & (tl >= 32) & (sI >= tI)), F_(same & (sI >= tI)) - F_(same & (sl >= 32)), F_(same & (sI < tI))], axis=1))
    pl_ = np.arange(128) % 64
    mPf, mPb = F_(pl_ <= 31), F_(pl_ >= 32)
    chA, chB = F_(np.arange(128) < 64), F_(np.arange(128) >= 64)
    sh["tokm"] = f(np.stack([np.stack([mPf, 1 - mPf, 0.125 * mPf, 0.125 * (1 - mPf), chA, chB], axis=1), np.stack([mPb, 1 - mPb, 0.125 * mPb, 0.125 * (1 - mPb), chA, chB], axis=1)], axis=1))
    sh["vmask"] = f(np.stack([same & (sI <= tI), same & (sI >= tI)], axis=1))
    pch = (np.arange(128)[:, None] // 64) == np.arange(2)[None, :]
    pl = (np.arange(128) % 64)[:, None]
    sh["ci"] = f(np.stack([np.concatenate([pch, pch & (pl <= 31)], axis=1), np.concatenate([pch, pch & (pl >= 32)], axis=1)], axis=1))
    sh["hg_g"] = f(inp["hg_norm_g"])
    sh["hg_lb"] = f(np.asarray(inp["hg_lb"]).reshape(1, -1))
    sh["ident"] = np.eye(128, dtype=np.float32)
    selm = np.zeros((NE, NE, 128), np.float32)
    for e in range(NE):
        selm[e, e, :] = 1.0
    sh["sel"] = selm
    return sh


def _prep_core(inp, b):
    f = lambda a: np.ascontiguousarray(np.asarray(a, dtype=np.float32))
    m = {}
    m["xin"] = f(np.concatenate([np.asarray(inp["x"])[b], np.asarray(inp["ctx"])[b]], axis=0))
    cc = np.stack([np.asarray(inp["c"])[b], np.asarray(inp["c_ctx"])], axis=1)
    m["cT"] = f(cc.reshape(8, 128, 2).transpose(1, 0, 2))
    return m


_NC_CACHE = {}


def kernel(**inputs):
    nc = build_nc()
    sh = _prep_shared(inputs)
    in_maps = []
    for b in range(8):
        m = dict(sh)
        m.update(_prep_core(inputs, b))
        in_maps.append(m)
    res = run_bass_kernel_spmd(nc, in_maps, core_ids=list(range(8)))
    out = np.stack([np.asarray(r["out"], dtype=np.float32) for r in res.results], axis=0)
    return out
```
